# Optimizing a Trainium2 kernel written in Bass

```python
import jax, jax.numpy as jnp
from jax import lax
import numpy as np

D_MODEL = 2048
BATCH = 4
SEQ = 2048
DEPTH = 2
DEC_BATCH = 128
DEC_SEQ = 8
PAST_LEN = 16384
PAGE_SIZE = 128

D_MIX = D_MODEL
D_POOL = D_MIX // 2
D_GATE = D_MIX - D_POOL
POOL_WINDOWS = (2, 4, 8, 16)
N_POOL_GROUPS = len(POOL_WINDOWS)
POOL_GROUP_DIM = D_POOL // N_POOL_GROUPS
POOL_BUF = max(POOL_WINDOWS) - 1
CHUNK = 128
N_GATE_HEADS = 8
GATE_HEAD_DIM = D_GATE // N_GATE_HEADS
D_IN = D_POOL + 2 * D_GATE
DFF_DENSE = ((8 * D_MODEL // 3 + 255) // 256) * 256
N_EXPERTS = 8
TOP_K = 2
DFF_EXPERT = 7 * D_MODEL // 2
N_DENSE = (DEPTH + 1) // 2
N_MOE = DEPTH // 2
EPS = 1e-6

kernel_name = "hybrid_pool_gmlp_decoder_step"


def rmsnorm(x, g):
    xf = x.astype(jnp.float32)
    y = xf * lax.rsqrt(jnp.mean(xf * xf, axis=-1, keepdims=True) + EPS)
    return (y * g.astype(jnp.float32)).astype(x.dtype)


def pool_mixer(p, prefix, start_pos, w_pool, pool_scale):
    b, l, _ = p.shape
    full = jnp.concatenate([prefix.astype(p.dtype), p], axis=1)
    cs = jnp.cumsum(full.astype(jnp.float32), axis=1)
    cs = jnp.concatenate([jnp.zeros((b, 1, D_POOL), jnp.float32), cs], axis=1)
    n_avail = start_pos + jnp.arange(l, dtype=jnp.int32) + 1
    means = []
    for gi, w in enumerate(POOL_WINDOWS):
        c0 = gi * POOL_GROUP_DIM
        c1 = c0 + POOL_GROUP_DIM
        hi = cs[:, POOL_BUF + 1:POOL_BUF + 1 + l, c0:c1]
        lo = cs[:, POOL_BUF + 1 - w:POOL_BUF + 1 - w + l, c0:c1]
        cnt = jnp.minimum(n_avail, w).astype(jnp.float32)[None, :, None]
        means.append((hi - lo) / cnt)
    r = jnp.concatenate(means, axis=-1) - p.astype(jnp.float32)
    r = r.astype(p.dtype).reshape(b, l, N_POOL_GROUPS, POOL_GROUP_DIM)
    out = jnp.einsum('blgc,gcd->blgd', r, w_pool).reshape(b, l, D_POOL) * pool_scale
    return out, full[:, -POOL_BUF:]


def spatial_gate(u, v, w_s, b_s, n_chunks, chunk_len):
    b = u.shape[0]
    vc = v.reshape(b, n_chunks, chunk_len, N_GATE_HEADS, GATE_HEAD_DIM)
    mask = jnp.tril(jnp.ones((CHUNK, CHUNK), w_s.dtype))
    ws = (w_s * mask)[:, :chunk_len, :chunk_len]
    bias = jnp.swapaxes(b_s[:, :chunk_len], 0, 1)[None, None, :, :, None]
    z = jnp.einsum('hts,bcshd->bcthd', ws, vc) + bias
    return u * z.reshape(u.shape)


def mixer_block(h, prefix, start_pos, n_chunks, chunk_len, g_mix, w_in, g_v, w_pool,
                pool_scale, w_s, b_s, w_out):
    xn = rmsnorm(h, g_mix)
    proj = jnp.einsum('bld,de->ble', xn, w_in)
    p = proj[..., :D_POOL]
    uv = jax.nn.gelu(proj[..., D_POOL:], approximate=False)
    u = uv[..., :D_GATE]
    v = rmsnorm(uv[..., D_GATE:], g_v)
    pool_out, buf = pool_mixer(p, prefix, start_pos, w_pool, pool_scale)
    gate_out = spatial_gate(u, v, w_s, b_s, n_chunks, chunk_len)
    y = jnp.einsum('ble,ed->bld', jnp.concatenate([pool_out, gate_out], axis=-1), w_out)
    return h + y, buf, v


def ffn_dense(h, g, wg, wu, wd):
    xn = rmsnorm(h, g)
    a = jax.nn.silu(xn @ wg) * (xn @ wu)
    return h + a @ wd


def ffn_moe(h, g, w_router, wg, wu, wd):
    b, l, d = h.shape
    xt = rmsnorm(h, g).reshape(b * l, d)
    logits = xt.astype(jnp.float32) @ w_router.astype(jnp.float32)
    vals, idx = lax.top_k(logits, TOP_K)
    gates = jax.nn.softmax(vals, axis=-1)
    combine = jnp.sum(jax.nn.one_hot(idx, N_EXPERTS, dtype=jnp.float32) * gates[..., None], axis=1)
    out = jnp.zeros((b * l, d), jnp.float32)
    for e in range(N_EXPERTS):
        a = jax.nn.silu(xt @ wg[e]) * (xt @ wu[e])
        out = out + combine[:, e:e + 1] * (a @ wd[e]).astype(jnp.float32)
    return h + out.astype(h.dtype).reshape(b, l, d)


def setup_inputs(seed: int = 0) -> dict:
    key = jax.random.key(seed)
    ks = jax.random.split(key, 24)

    def nrm(k, shape, scale):
        return jax.random.normal(k, shape, jnp.float32) * scale

    return {
        "x_prompt": nrm(ks[0], (BATCH, SEQ, D_MODEL), 1.0),
        "x_sample": nrm(ks[1], (DEC_BATCH, DEC_SEQ, D_MODEL), 1.0),
        "state_pool": nrm(ks[2], (DEPTH, DEC_BATCH, POOL_BUF, D_POOL), 1.0),
        "g_mix": 1.0 + nrm(ks[3], (DEPTH, D_MODEL), 0.02),
        "w_in": nrm(ks[4], (DEPTH, D_MODEL, D_IN), D_MODEL ** -0.5),
        "g_v": 1.0 + nrm(ks[5], (DEPTH, D_GATE), 0.02),
        "w_pool": nrm(ks[6], (DEPTH, N_POOL_GROUPS, POOL_GROUP_DIM, POOL_GROUP_DIM), POOL_GROUP_DIM ** -0.5),
        "pool_scale": 1.0 + nrm(ks[7], (DEPTH, D_POOL), 0.02),
        "w_s": nrm(ks[8], (DEPTH, N_GATE_HEADS, CHUNK, CHUNK), CHUNK ** -0.5),
        "b_s": 1.0 + nrm(ks[9], (DEPTH, N_GATE_HEADS, CHUNK), 0.02),
        "w_out": nrm(ks[10], (DEPTH, D_MIX, D_MODEL), D_MIX ** -0.5),
        "g_ffn": 1.0 + nrm(ks[11], (DEPTH, D_MODEL), 0.02),
        "dense_w_gate": nrm(ks[12], (N_DENSE, D_MODEL, DFF_DENSE), D_MODEL ** -0.5),
        "dense_w_up": nrm(ks[13], (N_DENSE, D_MODEL, DFF_DENSE), D_MODEL ** -0.5),
        "dense_w_down": nrm(ks[14], (N_DENSE, DFF_DENSE, D_MODEL), DFF_DENSE ** -0.5),
        "w_router": nrm(ks[15], (N_MOE, D_MODEL, N_EXPERTS), D_MODEL ** -0.5),
        "moe_w_gate": nrm(ks[16], (N_MOE, N_EXPERTS, D_MODEL, DFF_EXPERT), D_MODEL ** -0.5),
        "moe_w_up": nrm(ks[17], (N_MOE, N_EXPERTS, D_MODEL, DFF_EXPERT), D_MODEL ** -0.5),
        "moe_w_down": nrm(ks[18], (N_MOE, N_EXPERTS, DFF_EXPERT, D_MODEL), DFF_EXPERT ** -0.5),
        "g_final": 1.0 + nrm(ks[19], (D_MODEL,), 0.02),
    }


def reference(x_prompt, x_sample, state_pool, g_mix, w_in, g_v, w_pool, pool_scale, w_s, b_s,
              w_out, g_ffn, dense_w_gate, dense_w_up, dense_w_down, w_router, moe_w_gate,
              moe_w_up, moe_w_down, g_final):
    hp = x_prompt
    hs = x_sample
    n_chunks_p = hp.shape[1] // CHUNK
    zero_prefix = jnp.zeros((hp.shape[0], POOL_BUF, D_POOL), hp.dtype)
    bufs_p, bufs_s, vs_s = [], [], []
    for i in range(DEPTH):
        hp, bp, _ = mixer_block(hp, zero_prefix, 0, n_chunks_p, CHUNK, g_mix[i], w_in[i], g_v[i],
                                w_pool[i], pool_scale[i], w_s[i], b_s[i], w_out[i])
        hs, bsmp, v_s = mixer_block(hs, state_pool[i], PAST_LEN, 1, hs.shape[1], g_mix[i], w_in[i],
                                    g_v[i], w_pool[i], pool_scale[i], w_s[i], b_s[i], w_out[i])
        bufs_p.append(bp)
        bufs_s.append(bsmp)
        vs_s.append(v_s)
        j = i // 2
        if i % 2 == 0:
            hp = ffn_dense(hp, g_ffn[i], dense_w_gate[j], dense_w_up[j], dense_w_down[j])
            hs = ffn_dense(hs, g_ffn[i], dense_w_gate[j], dense_w_up[j], dense_w_down[j])
        else:
            hp = ffn_moe(hp, g_ffn[i], w_router[j], moe_w_gate[j], moe_w_up[j], moe_w_down[j])
            hs = ffn_moe(hs, g_ffn[i], w_router[j], moe_w_gate[j], moe_w_up[j], moe_w_down[j])
    y_prompt = rmsnorm(hp, g_final)
    y_sample = rmsnorm(hs, g_final)
    new_pool_prompt = jnp.stack(bufs_p)
    new_pool_sample = jnp.stack(bufs_s)
    new_v_sample = jnp.stack(vs_s)
    return (y_prompt, y_sample, new_pool_prompt, new_pool_sample, new_v_sample)
```

```python
import contextlib
import os
import numpy as np
import concourse.bass as bass
import concourse.mybir as mybir
from concourse.bass_utils import run_bass_kernel_spmd

F32, BF16 = mybir.dt.float32, mybir.dt.bfloat16
AF = mybir.ActivationFunctionType
ALU = mybir.AluOpType

D = 2048
KC = 16
TT = 1280
DP = 1024
DIN = 3072
DFF_D = 5632
DFF_E = 7168
NE = 8
EPS = 1e-6
NCORES = 8
WINS = (2, 4, 8, 16)
NSLOT = 5
ENGS = {"pe": "tensor", "act": "scalar", "dve": "vector", "pool": "gpsimd", "sp": "sync"}


class Op:
    __slots__ = ("eng", "fn", "deps", "signal", "dsem", "sem", "val", "inc")

    def __init__(self, eng, fn, dsem):
        self.eng, self.fn, self.dsem = eng, fn, dsem
        self.deps, self.signal, self.sem, self.val, self.inc = [], False, None, 0, 1


class DSem:
    def __init__(self, h):
        self.h, self.count = h, 0


class Prog:
    def __init__(self, nc, es):
        self.nc, self.es = nc, es
        self.ops, self.last_w, self.readers = [], {}, {}
        self.nsem = 0
        self.phase_op = None
        self.phase_idx = 0
        self.bar_tile = None

    def new_sem(self):
        self.nsem += 1
        return self.es.enter_context(self.nc.semaphore("s%d" % self.nsem))

    def dsem(self):
        return DSem(self.new_sem())

    def add(self, eng, fn, reads=(), writes=(), dsem=None):
        op = Op(eng, fn, dsem)
        deps = []
        seen = set()

        def dep(o):
            if o is None or id(o) in seen:
                return
            seen.add(id(o))
            if o.eng == "pe" and eng == "pe" and o.dsem is None:
                return
            deps.append(o)
            o.signal = True

        dep(self.phase_op)
        for k in reads:
            dep(self.last_w.get(k))
        for k in writes:
            dep(self.last_w.get(k))
            for r in self.readers.get(k, ()):
                dep(r)
        op.deps = deps
        for k in reads:
            self.readers.setdefault(k, []).append(op)
        for k in writes:
            self.last_w[k] = op
            self.readers[k] = []
        self.ops.append(op)
        return op

    def barrier(self):
        deps, seen_eng = [], set()
        for o in reversed(self.ops[self.phase_idx:]):
            if o.dsem is not None:
                deps.append(o)
            elif o.eng not in seen_eng:
                seen_eng.add(o.eng)
                deps.append(o)
        bt = self.bar_tile
        op = Op("dve", lambda e: e.memset(bt[:, :], 0.0), None)
        if self.phase_op is not None:
            deps.append(self.phase_op)
        for d in deps:
            d.signal = True
        op.deps = deps
        self.ops.append(op)
        self.phase_op = op
        self.phase_idx = len(self.ops)

    def assign(self):
        cnt, cur = {}, {}
        for op in self.ops:
            if not op.signal:
                continue
            if op.dsem is not None:
                op.dsem.count += 16
                op.sem, op.val, op.inc = op.dsem.h, op.dsem.count, 16
            else:
                e = op.eng
                if e not in cur or cnt[e] >= 30000:
                    cur[e], cnt[e] = self.new_sem(), 0
                cnt[e] += 1
                op.sem, op.val, op.inc = cur[e], cnt[e], 1

    def finalize(self, block):
        per = {e: [] for e in ENGS}
        for op in self.ops:
            per[op.eng].append(op)
        for e, lst in per.items():
            def body(eng, lst=lst):
                waited = {}
                for op in lst:
                    need = {}
                    for d in op.deps:
                        k = id(d.sem)
                        if need.get(k, (None, 0))[1] < d.val:
                            need[k] = (d.sem, d.val)
                    for k, (sm, v) in need.items():
                        if waited.get(k, 0) >= v:
                            continue
                        eng.wait_ge(sm, v)
                        waited[k] = v
                    ins = op.fn(eng)
                    if op.signal:
                        ins.then_inc(op.sem, op.inc)
            getattr(block, ENGS[e])(body)


def build_program(stage=9):
    nc = bass.Bass("TRN2", target_bir_lowering=False)

    def din(name, shape):
        return nc.dram_tensor(name, list(shape), F32, kind="ExternalInput").ap()

    def dout(name, shape):
        return nc.dram_tensor(name, list(shape), F32, kind="ExternalOutput").ap()

    xin = din("xin", [TT, D])
    spool = din("spool", [2, 240, DP])
    w_in = din("w_in", [2, D, DIN])
    w_out = din("w_out", [2, D, D])
    w_pool = din("w_pool", [2, 4, 256, 256])
    dg = din("dg", [D, DFF_D])
    du = din("du", [D, DFF_D])
    dd = din("dd", [DFF_D, D])
    if stage >= 5:
        mg = din("mg", [NE, D, DFF_E])
        mu = din("mu", [NE, D, DFF_E])
        md = din("md", [NE, DFF_E, D])
    wr = din("wr", [D, NE])
    wsT = din("wsT", [2, 128, 8, 128])
    wsbig = din("wsbig", [2, 128, 8, 128])
    cvec_d = din("cvec", [128, 96])
    gvbc_d = din("gvbc", [2, 128, DP])
    bsrow_d = din("bsrow", [2, 1, 2048])
    cmat_d = din("cmat", [128, 27, 128])
    identf_d = din("identf", [128, 128])
    sel_d = din("sel", [8, 1024])
    yout = dout("yout", [1152, D])
    npp = dout("npp", [2, 15, DP])
    nps_a = dout("nps_a", [2, 16, 7, DP])
    nps_b = dout("nps_b", [2, 128, DP])
    nvs = dout("nvs", [2, 128, DP])
    DBG = os.environ.get("DBG_DUMP")
    if DBG:
        dbg1 = nc.dram_tensor("dbg1", [128, 4096], BF16, kind="ExternalOutput").ap()
        dbg2 = dout("dbg2", [128, 256])

    with contextlib.ExitStack() as es:
        def sb(name, shape, dt):
            return es.enter_context(nc.sbuf_tensor("sb_" + name, list(shape), dt))

        P = Prog(nc, es)
        h = sb("h", [128, KC, TT], F32)
        ring = [sb("ring%d" % i, [128, 4096], BF16) for i in range(NSLOT)]
        ovl = sb("ovl", [128, 12800], F32)
        ovl2 = sb("ovl2", [128, 8192], F32)
        cvec = sb("cvec", [128, 96], F32)
        identf = sb("identf", [128, 128], F32)
        onesf = sb("onesf", [1, 128], F32)
        onesb = sb("onesb", [128, 128], BF16)
        epsc = sb("epsc", [128, 1], F32)
        small = sb("small", [128, 64], F32)
        P.bar_tile = sb("bart", [128, 2], F32)
        psall = es.enter_context(nc.psum_tensor("psall", [128, 4096], F32))
        ps = [psall[:, i * 512:(i + 1) * 512] for i in range(8)]
        dstate = {"i": 0}

        def view(reg, off, dt, n):
            w = 4 if dt == F32 else 2
            a = reg[:, off // 4:(off + n * w) // 4]
            return a if dt == F32 else a.bitcast(BF16)

        xn = view(ovl, 0, BF16, KC * TT).rearrange("p (k n) -> p k n", k=KC)
        abuf = view(ovl, 40960, BF16, 4 * TT).rearrange("p (k n) -> p k n", k=4)
        xstage = view(ovl, 0, F32, 2 * D).rearrange("p (b n) -> p b n", b=2)
        ytmp = view(ovl, 16384, F32, 512).rearrange("p (j n) -> p j n", j=4)
        xn_t = view(ovl, 0, BF16, KC * 256).rearrange("p (k n) -> p k n", k=KC)
        pbf = view(ovl, 8192, BF16, 3 * DP).rearrange("p (q n) -> p q n", q=3)
        pf32 = view(ovl, 14336, F32, 2 * DP).rearrange("p (q n) -> p q n", q=2)
        vg = view(ovl, 22528, F32, DP)
        vnbf = view(ovl, 26624, BF16, 2 * DP).rearrange("p (q n) -> p q n", q=2)
        ubuf = view(ovl, 30720, BF16, 8 * 256).rearrange("p (k n) -> p k n", k=8)
        rbuf = view(ovl, 34816, BF16, 8 * 256).rearrange("p (k n) -> p k n", k=8)
        cat = view(ovl, 38912, BF16, KC * 256).rearrange("p (k n) -> p k n", k=KC)
        junk = view(ovl, 47104, BF16, DP)
        sq4m = junk.rearrange("p (k n) -> p k n", k=4)
        cmat = view(ovl2, 0, BF16, 27 * 128).rearrange("p (m n) -> p m n", m=27)
        wsTm = view(ovl2, 6912, BF16, 1024).rearrange("p (h n) -> p h n", h=8)
        wsbm = view(ovl2, 8960, BF16, 1024).rearrange("p (h n) -> p h n", h=8)
        gvbc = view(ovl2, 11008, F32, DP)
        wpool = view(ovl2, 15104, BF16, 2048).rearrange("p (g k n) -> p g k n", g=4, k=2)
        bsrow = view(ovl2, 19200, F32, 2048)
        spre = view(ovl2, 27392, BF16, 2 * DP).rearrange("p (j n) -> p j n", j=2)
        rstd = view(ovl2, 0, F32, TT)
        selt = view(ovl2, 5120, F32, 1024).rearrange("p (e n) -> p e n", e=8)
        combT = view(ovl2, 9216, F32, 1152)
        gbc = view(ovl2, 13824, F32, 2 * 1152).rearrange("p (b n) -> p b n", b=2)
        wrg = view(ovl2, 23040, F32, KC * 8).rearrange("p (k e) -> p k e", k=KC)
        silt = view(ovl2, 23552, BF16, 2 * 416).rearrange("p (b n) -> p b n", b=2)
        tmp2 = view(ovl2, 25216, F32, 416)
        rstd_m = view(ovl2, 31488, F32, 256)

        def c_gmix(l, k): return cvec[:, l * 16 + k:l * 16 + k + 1]
        def c_psc(l, k): return cvec[:, 32 + l * 8 + k:32 + l * 8 + k + 1]
        def c_gffn(l, k): return cvec[:, 48 + l * 16 + k:48 + l * 16 + k + 1]
        def c_gfin(k): return cvec[:, 80 + k:80 + k + 1]

        rot = {"lo": 0, "hi": 0, "all": 0}

        def bank(which):
            if which == "hi":
                b = 4 + rot["hi"] % 4
                rot["hi"] += 1
            else:
                b = rot["all"] % 8
                rot["all"] += 1
            return b

        rstate = {"i": 0}
        rsems = [P.dsem() for _ in range(NSLOT)]

        def ring_load(src3d, shape3, mdl=4096):
            s = rstate["i"] % NSLOT
            rstate["i"] += 1
            dst = ring[s][:, :].rearrange("p (a b) -> p a b", a=shape3[0])
            P.add("pool", lambda e: e.dma_start(out=dst, in_=src3d, max_dma_last_dim=mdl), writes=[("ring", s)], dsem=rsems[s])
            return s, dst

        def wblock(w2d, c0):
            return ring_load(w2d.rearrange("(k p) n -> p k n", p=128)[:, :, c0:c0 + 256], (16, 256))

        def dblock(w2d, r0):
            return ring_load(w2d[r0:r0 + 256, :].rearrange("(k p) n -> p k n", p=128), (2, 2048), 8192)

        s_c = P.dsem()
        P.add("sp", lambda e: e.dma_start(out=cvec[:, :], in_=cvec_d[:, :]), writes=["cvec"], dsem=s_c)
        s_i = P.dsem()
        P.add("sp", lambda e: e.dma_start(out=identf[:, :], in_=identf_d[:, :]), writes=["identf"], dsem=s_i)
        P.add("dve", lambda e: e.memset(onesf[:, :], 1.0), writes=["onesf"])
        P.add("dve", lambda e: e.memset(onesb[:, :], 1.0), writes=["onesb"])
        P.add("dve", lambda e: e.memset(epsc[:, :], EPS), writes=["epsc"])

        s_x = [P.dsem(), P.dsem()]
        flip = [0]

        def evac(out_ap, in_ap, reads, writes):
            flip[0] ^= 1
            if flip[0]:
                P.add("act", lambda e: e.activation(out=out_ap, in_=in_ap, func=AF.Copy), reads=reads, writes=writes)
            else:
                P.add("dve", lambda e: e.tensor_copy(out=out_ap, in_=in_ap), reads=reads, writes=writes)

        for q in range(10):
            sl = q % 2
            P.add("sp", lambda e, q=q, sl=sl: e.dma_start(out=xstage[:, sl, :], in_=xin[q * 128:(q + 1) * 128, :]),
                  writes=[("xs", sl)], dsem=s_x[sl])
            for c4 in range(4):
                b = bank("all")

                def tr(e, b=b, sl=sl, c4=c4):
                    ins = None
                    for j in range(4):
                        c = c4 * 4 + j
                        ins = e.transpose(out=ps[b][:, j * 128:(j + 1) * 128], in_=xstage[:, sl, c * 128:(c + 1) * 128],
                                          identity=identf[:, :])
                    return ins
                P.add("pe", tr, reads=[("xs", sl), "identf"], writes=[("ps", b)])
                evac(h[:, c4 * 4:c4 * 4 + 4, q * 128:(q + 1) * 128], ps[b][:, :].rearrange("p (j n) -> p j n", j=4),
                     reads=[("ps", b)], writes=[("h", c4 * 4 + j, q) for j in range(4)])

        def hkeys(k, c0, c1):
            return [("h", k, q) for q in range(c0 // 128, (c1 + 127) // 128)]

        def norm_stage(c0, c1, sq4, sqkey, rs, rskey, gcol, xn_out, xnkey, do_xn=True):
            W = c1 - c0
            b = bank("hi")
            for kk in range(4):
                P.add("act", lambda e, kk=kk: e.activation(out=sq4[:, :, 0:W], in_=h[:, kk * 4:kk * 4 + 4, c0:c1], func=AF.Square),
                      reads=sum([hkeys(kk * 4 + j, c0, c1) for j in range(4)], []), writes=[sqkey])

                def mm(e, kk=kk):
                    ins = None
                    for j in range(4):
                        ins = e.matmul(ps[b][:, 0:W], lhsT=onesb[:, :], rhs=sq4[:, j, 0:W],
                                       start=(kk == 0 and j == 0), stop=(kk == 3 and j == 3))
                    return ins
                P.add("pe", mm, reads=[sqkey, "onesb"], writes=[("ps", b)])
            P.add("act", lambda e: e.activation(out=rs[:, 0:W], in_=ps[b][:, 0:W], func=AF.Sqrt, bias=epsc[:, 0:1], scale=1.0 / D),
                  reads=[("ps", b), "epsc"], writes=[rskey])
            P.add("dve", lambda e: e.reciprocal(out=rs[:, 0:W], in_=rs[:, 0:W]), reads=[rskey], writes=[rskey])
            if do_xn:
                for k in range(KC):
                    P.add("dve", lambda e, k=k: e.scalar_tensor_tensor(out=xn_out[:, k, 0:W], in0=h[:, k, c0:c1], scalar=gcol(k),
                                                                    in1=rs[:, 0:W], op0=ALU.mult, op1=ALU.mult),
                          reads=hkeys(k, c0, c1) + [rskey, "cvec"], writes=[(xnkey, k)])

        s_l = [P.dsem() for _ in range(7)]
        s_o = [P.dsem() for _ in range(4)]

        def mixer(l):
            SM = int(os.environ.get("SETUP_MASK", "511"))
            if SM & 1:
                P.add("pool", lambda e: e.dma_start(out=cmat, in_=cmat_d[:, :, :], max_dma_last_dim=4096), writes=["cmat"], dsem=s_l[0])
            if SM & 2:
                P.add("pool", lambda e: e.dma_start(out=wsTm, in_=wsT[l], max_dma_last_dim=4096), writes=["wsTm"], dsem=s_l[1])
            if SM & 4:
                P.add("pool", lambda e: e.dma_start(out=wsbm, in_=wsbig[l], max_dma_last_dim=4096), writes=["wsbm"], dsem=s_l[2])
            if SM & 8:
                P.add("pool", lambda e: e.dma_start(out=wpool, in_=w_pool[l].rearrange("g (k p) n -> p g k n", p=128), max_dma_last_dim=4096),
                      writes=["wpool"], dsem=s_l[3])
            if SM & 16:
                P.add("pool", lambda e: e.dma_start(out=spre[0:120, :, :], in_=spool[l].rearrange("(j r) n -> r j n", j=2), max_dma_last_dim=4096),
                      writes=["spre"], dsem=s_l[4])
            if SM & 32:
                P.add("sp", lambda e: e.dma_start(out=gvbc, in_=gvbc_d[l]), writes=["gvbc"], dsem=s_l[5])
            if SM & 64:
                P.add("sp", lambda e: e.dma_start(out=bsrow[0:1, :], in_=bsrow_d[l]), writes=["bsrow"], dsem=s_l[6])
            for hh in (range(8) if SM & 128 else []):
                P.add("dve", lambda e, hh=hh: e.tensor_tensor(out=wsTm[:, hh, :], in0=wsTm[:, hh, :], in1=cmat[:, 1, :], op=ALU.mult),
                      reads=["wsTm", "cmat"], writes=["wsTm"])
                P.add("dve", lambda e, hh=hh: e.tensor_tensor(out=wsbm[:, hh, :], in0=wsbm[:, hh, :], in1=cmat[:, 2, :], op=ALU.mult),
                      reads=["wsbm", "cmat"], writes=["wsbm"])
            if SM & 256:
                P.add("sp", lambda e: e.dma_start(out=nps_a[l], in_=spool[l].rearrange("(b r) n -> b r n", r=15)[:, 8:15, :]),
                      writes=[("nps_a", l)], dsem=s_o[0])

            MS = int(os.environ.get("MIX_STEPS", "99"))
            for ti in range(int(os.environ.get("MIX_TILES", "5"))):
                c0 = ti * 256
                qa = 2 * ti
                if MS < 1:
                    continue
                norm_stage(c0, c0 + 256, sq4m, "junk", rstd_m, "rstd_m", lambda k: c_gmix(l, k), xn_t, "xn_t")
                xnk = [("xn_t", k) for k in range(KC)]

                def tokmajor(cbase):
                    for bq in range(4):
                        s, wv = wblock(w_in[l], cbase + bq * 256)
                        for qi in range(2):
                            bnk = 2 * qi + bq // 2

                            def mm(e, wv=wv, qi=qi, bnk=bnk, bq=bq):
                                ins = None
                                for k in range(KC):
                                    ins = e.matmul(ps[bnk][:, (bq % 2) * 256:(bq % 2) * 256 + 256], lhsT=xn_t[:, k, qi * 128:(qi + 1) * 128],
                                                   rhs=wv[:, k, :], start=(k == 0), stop=(k == KC - 1))
                                return ins
                            P.add("pe", mm, reads=xnk + [("ring", s)], writes=[("ps", bnk)])

                if DBG and ti == 4 and l == 0:
                    sd1, sd2 = P.dsem(), P.dsem()
                    P.add("sp", lambda e: e.dma_start(out=dbg1, in_=xn_t.rearrange("p k n -> p (k n)")), reads=xnk, writes=["dbg1"], dsem=sd1)
                    P.add("sp", lambda e: e.dma_start(out=dbg2, in_=rstd_m), reads=["rstd_m"], writes=["dbg2"], dsem=sd2)
                if MS < 2:
                    continue
                tokmajor(0)
                for qi in range(2):
                    q = qa + qi
                    for hf in range(2):
                        bnk = 2 * qi + hf
                        P.add("act", lambda e, q=q, hf=hf, bnk=bnk: e.activation(out=pbf[:, q % 3, hf * 512:(hf + 1) * 512], in_=ps[bnk][:, :], func=AF.Copy),
                              reads=[("ps", bnk)], writes=[("pbf", q % 3)])
                        if q >= 8 and not os.environ.get("NO_PF32"):
                            P.add("act", lambda e, q=q, hf=hf, bnk=bnk: e.activation(out=pf32[:, q - 8, hf * 512:(hf + 1) * 512], in_=ps[bnk][:, :], func=AF.Copy),
                                  reads=[("ps", bnk)], writes=[("pf32", q - 8)])
                    if q == 8 and not os.environ.get("NO_NPP"):
                        P.add("sp", lambda e: e.dma_start(out=npp[l], in_=pf32[113:128, 0, :]), reads=[("pf32", 0)], writes=[("npp", l)], dsem=s_o[1])
                    if q == 9 and not os.environ.get("NO_NPSB"):
                        P.add("sp", lambda e: e.dma_start(out=nps_b[l], in_=pf32[:, 1, :]), reads=[("pf32", 1)], writes=[("nps_b", l)], dsem=s_o[2])
                if DBG and ti == int(os.environ.get("DBG_TI", "0")) and l == 0:
                    sd3 = P.dsem()
                    dbg3 = nc.dram_tensor("dbg3", [128, 3072], BF16, kind="ExternalOutput").ap()
                    P.add("sp", lambda e: e.dma_start(out=dbg3, in_=pbf.rearrange("p q n -> p (q n)")), reads=[("pbf", i) for i in range(3)], writes=["dbg3"], dsem=sd3)
                if MS < 3:
                    continue
                for bq in range(4):
                    s, wv = wblock(w_in[l], DP + bq * 256)
                    for m in range(2):
                        bnk = bank("hi")

                        def mm(e, wv=wv, m=m, bnk=bnk):
                            ins = None
                            for k in range(KC):
                                ins = e.matmul(ps[bnk][:, 0:256], lhsT=wv[:, k, m * 128:(m + 1) * 128], rhs=xn_t[:, k, :],
                                               start=(k == 0), stop=(k == KC - 1))
                            return ins
                        P.add("pe", mm, reads=xnk + [("ring", s)], writes=[("ps", bnk)])
                        P.add("act", lambda e, bq=bq, m=m, bnk=bnk: e.activation(out=ubuf[:, bq * 2 + m, :], in_=ps[bnk][:, 0:256], func=AF.Gelu),
                              reads=[("ps", bnk)], writes=[("u", bq * 2 + m)])
                if MS < 4:
                    continue
                tokmajor(2 * DP)
                for qi in range(2):
                    q = qa + qi
                    for hf in range(2):
                        bnk = 2 * qi + hf
                        P.add("act", lambda e, hf=hf, bnk=bnk: e.activation(out=vg[:, hf * 512:(hf + 1) * 512], in_=ps[bnk][:, :], func=AF.Gelu),
                              reads=[("ps", bnk)], writes=["vg"])
                    P.add("dve", lambda e: e.scalar_tensor_tensor(out=junk, in0=vg, scalar=1.0, in1=vg, op0=ALU.mult, op1=ALU.mult,
                                                                  accum_out=small[:, 0:1]),
                          reads=["vg"], writes=["junk", "ss"])
                    P.add("act", lambda e: e.activation(out=small[:, 1:2], in_=small[:, 0:1], func=AF.Sqrt, bias=epsc[:, 0:1], scale=1.0 / DP),
                          reads=["ss", "epsc"], writes=["ss1"])
                    P.add("dve", lambda e: e.reciprocal(out=small[:, 2:3], in_=small[:, 1:2]), reads=["ss1"], writes=["ss2"])
                    if q < 9:
                        P.add("dve", lambda e, q=q: e.scalar_tensor_tensor(out=vnbf[:, q % 2, :], in0=vg, scalar=small[:, 2:3], in1=gvbc,
                                                                        op0=ALU.mult, op1=ALU.mult),
                              reads=["vg", "ss2", "gvbc"], writes=[("vnbf", q % 2)])
                    else:
                        P.add("dve", lambda e: e.scalar_tensor_tensor(out=vg, in0=vg, scalar=small[:, 2:3], in1=gvbc, op0=ALU.mult, op1=ALU.mult),
                              reads=["vg", "ss2", "gvbc"], writes=["vg"])
                        P.add("act", lambda e, q=q: e.activation(out=vnbf[:, q % 2, :], in_=vg, func=AF.Copy), reads=["vg"], writes=[("vnbf", q % 2)])
                        P.add("sp", lambda e: e.dma_start(out=nvs[l], in_=vg), reads=["vg"], writes=[("nvs", l)], dsem=s_o[3])

                if MS < 5:
                    continue
                for qi in range(2):
                    q = qa + qi
                    for half in range(2):
                        bnk = bank("hi")

                        def mm(e, q=q, half=half, bnk=bnk):
                            ins = None
                            for j in range(4):
                                fc = half * 4 + j
                                g = fc // 2
                                o = ps[bnk][:, j * 128:(j + 1) * 128]
                                lt = pbf[:, q % 3, fc * 128:(fc + 1) * 128]
                                if q == 9:
                                    e.matmul(o, lhsT=lt, rhs=cmat[:, 3 + g * 6 + 3, :], start=True, stop=False)
                                    e.matmul(o, lhsT=spre[0:120, 0, fc * 128:(fc + 1) * 128], rhs=cmat[0:120, 3 + g * 6 + 4, :], start=False, stop=False)
                                    ins = e.matmul(o, lhsT=spre[0:120, 1, fc * 128:(fc + 1) * 128], rhs=cmat[0:120, 3 + g * 6 + 5, :], start=False, stop=True)
                                elif q == 0:
                                    ins = e.matmul(o, lhsT=lt, rhs=cmat[:, 3 + g * 6 + 0, :], start=True, stop=True)
                                else:
                                    e.matmul(o, lhsT=lt, rhs=cmat[:, 3 + g * 6 + (2 if q == 1 else 0), :], start=True, stop=False)
                                    ins = e.matmul(o, lhsT=pbf[:, (q - 1) % 3, fc * 128:(fc + 1) * 128], rhs=cmat[:, 3 + g * 6 + 1, :],
                                                   start=False, stop=True)
                            return ins
                        rd = [("pbf", q % 3), "cmat"] + ([("pbf", (q - 1) % 3)] if 1 <= q <= 8 else []) + (["spre"] if q == 9 else [])
                        P.add("pe", mm, reads=rd, writes=[("ps", bnk)])
                        evac(rbuf[:, half * 4:half * 4 + 4, qi * 128:(qi + 1) * 128], ps[bnk][:, :].rearrange("p (j n) -> p j n", j=4),
                             reads=[("ps", bnk)], writes=[("r", half * 4 + j, qi) for j in range(4)])
                for fo in range(8):
                    g, m = fo // 2, fo % 2
                    bnk = bank("hi")

                    def mm(e, g=g, m=m, bnk=bnk):
                        e.matmul(ps[bnk][:, 0:256], lhsT=wpool[:, g, 0, m * 128:(m + 1) * 128], rhs=rbuf[:, 2 * g, :], start=True, stop=False)
                        return e.matmul(ps[bnk][:, 0:256], lhsT=wpool[:, g, 1, m * 128:(m + 1) * 128], rhs=rbuf[:, 2 * g + 1, :], start=False, stop=True)
                    P.add("pe", mm, reads=["wpool"] + [("r", 2 * g + kk, qi) for kk in range(2) for qi in range(2)], writes=[("ps", bnk)])
                    P.add("act", lambda e, fo=fo, bnk=bnk: e.activation(out=cat[:, fo, :], in_=ps[bnk][:, 0:256], func=AF.Copy, scale=c_psc(l, fo)),
                          reads=[("ps", bnk), "cvec"], writes=[("cat", fo)])
                if MS < 6:
                    continue
                for qi in range(2):
                    q = qa + qi
                    for hh in (0, 4):
                        bnk = bank("hi")

                        def mm(e, q=q, hh=hh, bnk=bnk):
                            ins = None
                            for j in range(4):
                                hd = hh + j
                                o = ps[bnk][:, j * 128:(j + 1) * 128]
                                wm = wsbm if q == 9 else wsTm
                                boff = (1024 if q == 9 else 0) + hd * 128
                                e.matmul(o, lhsT=vnbf[:, q % 2, hd * 128:(hd + 1) * 128], rhs=wm[:, hd, :], start=True, stop=False)
                                ins = e.matmul(o, lhsT=onesf[0:1, :], rhs=bsrow[0:1, boff:boff + 128], start=False, stop=True)
                            return ins
                        P.add("pe", mm, reads=[("vnbf", q % 2), "wsTm", "wsbm", "bsrow", "onesf"], writes=[("ps", bnk)])
                        P.add("dve", lambda e, hh=hh, qi=qi, bnk=bnk: e.tensor_tensor(
                            out=cat[:, 8 + hh:12 + hh, qi * 128:(qi + 1) * 128], in0=ubuf[:, hh:hh + 4, qi * 128:(qi + 1) * 128],
                            in1=ps[bnk][:, :].rearrange("p (j n) -> p j n", j=4), op=ALU.mult),
                            reads=[("ps", bnk)] + [("u", hh + j) for j in range(4)], writes=[("cat", 8 + hh + j) for j in range(4)])
                if MS < 7:
                    continue
                catk = [("cat", k) for k in range(KC)]
                for bq in range(8):
                    s, wv = wblock(w_out[l], bq * 256)
                    for m in range(2):
                        bnk = bank("hi")
                        dc = bq * 2 + m

                        def mm(e, wv=wv, m=m, bnk=bnk):
                            ins = None
                            for k in range(KC):
                                ins = e.matmul(ps[bnk][:, 0:256], lhsT=wv[:, k, m * 128:(m + 1) * 128], rhs=cat[:, k, :],
                                               start=(k == 0), stop=(k == KC - 1))
                            return ins
                        P.add("pe", mm, reads=catk + [("ring", s)], writes=[("ps", bnk)])
                        P.add("dve", lambda e, dc=dc, bnk=bnk, c0=c0: e.tensor_tensor(out=h[:, dc, c0:c0 + 256], in0=h[:, dc, c0:c0 + 256],
                                                                             in1=ps[bnk][:, 0:256], op=ALU.add),
                              reads=[("ps", bnk)] + hkeys(dc, c0, c0 + 256), writes=hkeys(dc, c0, c0 + 256))

        def ffn_norm(l, tiles):
            for (c0, c1) in tiles:
                sq4 = abuf[:, :, 0:416]
                norm_stage(c0, c1, sq4, "abuf", rstd[:, c0:c1], ("rstd", c0), lambda k: c_gffn(l, k), xn[:, :, c0:c1], ("xn", c0))

        def ffn_body(tiles, wg2d, wu2d, wd2d, nchunks, gsel):
            for gi in range(nchunks // 4):
                f0 = gi * 4
                for hb in range(2):
                    sg, wgv = wblock(wg2d, (f0 + hb * 2) * 128)
                    su, wuv = wblock(wu2d, (f0 + hb * 2) * 128)
                    for fi2 in range(2):
                        fi = hb * 2 + fi2
                        for ti, (c0, c1) in enumerate(tiles):
                            W = c1 - c0
                            bg, bu = bank("all"), bank("all")
                            xk = [(("xn", c0), k) for k in range(KC)]

                            def mmg(e, wgv=wgv, fi2=fi2, bg=bg, c0=c0, c1=c1, W=W):
                                ins = None
                                for k in range(KC):
                                    ins = e.matmul(ps[bg][:, 0:W], lhsT=wgv[:, k, fi2 * 128:(fi2 + 1) * 128], rhs=xn[:, k, c0:c1],
                                                   start=(k == 0), stop=(k == KC - 1))
                                return ins

                            def mmu(e, wuv=wuv, fi2=fi2, bu=bu, c0=c0, c1=c1, W=W):
                                ins = None
                                for k in range(KC):
                                    ins = e.matmul(ps[bu][:, 0:W], lhsT=wuv[:, k, fi2 * 128:(fi2 + 1) * 128], rhs=xn[:, k, c0:c1],
                                                   start=(k == 0), stop=(k == KC - 1))
                                return ins
                            P.add("pe", mmg, reads=xk + [("ring", sg)], writes=[("ps", bg)])
                            P.add("pe", mmu, reads=xk + [("ring", su)], writes=[("ps", bu)])
                            sb_ = (fi * 3 + ti) % 2
                            P.add("act", lambda e, bg=bg, W=W, sb_=sb_: e.activation(out=silt[:, sb_, 0:W], in_=ps[bg][:, 0:W], func=AF.Silu),
                                  reads=[("ps", bg)], writes=[("silt", sb_)])
                            if gsel is None:
                                P.add("dve", lambda e, bu=bu, W=W, sb_=sb_, fi=fi, c0=c0, c1=c1: e.tensor_tensor(
                                    out=abuf[:, fi, c0:c1], in0=silt[:, sb_, 0:W], in1=ps[bu][:, 0:W], op=ALU.mult),
                                    reads=[("ps", bu), ("silt", sb_)], writes=[("a", fi, c0)])
                            else:
                                P.add("dve", lambda e, bu=bu, W=W, sb_=sb_: e.tensor_tensor(
                                    out=tmp2[:, 0:W], in0=silt[:, sb_, 0:W], in1=ps[bu][:, 0:W], op=ALU.mult),
                                    reads=[("ps", bu), ("silt", sb_)], writes=["tmp2"])
                                P.add("dve", lambda e, W=W, fi=fi, c0=c0, c1=c1: e.tensor_tensor(
                                    out=abuf[:, fi, c0:c1], in0=tmp2[:, 0:W], in1=gbc[:, gsel, c0 - 128:c1 - 128], op=ALU.mult),
                                    reads=["tmp2", ("gbc", gsel)], writes=[("a", fi, c0)])
                sd = [dblock(wd2d, (f0 + hb * 2) * 128) for hb in range(2)]
                for dc4 in range(4):
                    for (c0, c1) in tiles:
                        W = c1 - c0
                        b0 = 4 * (dstate["i"] % 2)
                        dstate["i"] += 1
                        for i4 in range(4):
                            dc = dc4 * 4 + i4
                            bnk = b0 + i4

                            def mmd(e, dc=dc, bnk=bnk, c0=c0, c1=c1, W=W, sd=sd):
                                ins = None
                                for fi in range(4):
                                    ins = e.matmul(ps[bnk][:, 0:W], lhsT=sd[fi // 2][1][:, fi % 2, dc * 128:(dc + 1) * 128], rhs=abuf[:, fi, c0:c1],
                                                   start=(fi == 0), stop=(fi == 3))
                                return ins
                            P.add("pe", mmd, reads=[("a", fi, c0) for fi in range(4)] + [("ring", sd[0][0]), ("ring", sd[1][0])], writes=[("ps", bnk)])
                        hk = sum([hkeys(dc4 * 4 + i4, c0, c1) for i4 in range(4)], [])
                        P.add("dve", lambda e, dc4=dc4, b0=b0, c0=c0, c1=c1, W=W: e.tensor_tensor(
                            out=h[:, dc4 * 4:dc4 * 4 + 4, c0:c1], in0=h[:, dc4 * 4:dc4 * 4 + 4, c0:c1],
                            in1=psall[:, b0 * 512:(b0 + 4) * 512].rearrange("p (b n) -> p b n", b=4)[:, :, 0:W], op=ALU.add),
                            reads=[("ps", b0 + i4) for i4 in range(4)] + hk, writes=hk)

        T0 = [(96, 512), (512, 896), (896, 1280)]
        T1 = [(128, 512), (512, 896), (896, 1280)]

        P.barrier()
        if stage >= 1:
            mixer(0)
        P.barrier()
        if stage >= 2:
            ffn_norm(0, T0)
            ffn_body(T0, dg, du, dd, DFF_D // 128, None)
        P.barrier()
        if stage >= 3:
            mixer(1)
        P.barrier()
        ffn_norm(1, T1)
        s_r = [P.dsem(), P.dsem()]
        P.add("sp", lambda e: e.dma_start(out=wrg, in_=wr.rearrange("(k p) e -> p k e", p=128)), writes=["wrg"], dsem=s_r[0])
        P.add("sp", lambda e: e.dma_start(out=selt[0:8, :, :], in_=sel_d.rearrange("k (e n) -> k e n", e=8)), writes=["selt"], dsem=s_r[1])
        for k in range(KC):
            P.add("dve", lambda e, k=k: e.tensor_scalar(out=wrg[:, k, :], in0=wrg[:, k, :], scalar1=c_gffn(1, k), scalar2=None, op0=ALU.mult),
                  reads=["wrg", "cvec"], writes=["wrg"])
        for q in (range(1, 10) if stage >= 4 else []):
            c0 = q * 128
            tile0 = 128 if q < 4 else (512 if q < 7 else 896)
            b1 = bank("all")

            def mmr(e, b1=b1, c0=c0):
                ins = None
                for k in range(KC):
                    ins = e.matmul(ps[b1][:, 0:8], lhsT=h[:, k, c0:c0 + 128], rhs=wrg[:, k, :], start=(k == 0), stop=(k == KC - 1))
                return ins
            P.add("pe", mmr, reads=["wrg"] + [("h", k, q) for k in range(KC)], writes=[("ps", b1)])
            b2 = bank("all")
            P.add("pe", lambda e, b2=b2, c0=c0: e.matmul(ps[b2][:, 0:1], lhsT=rstd[0:1, c0:c0 + 128], rhs=onesf[0:1, 0:1], start=True, stop=True),
                  reads=[("rstd", tile0), "onesf"], writes=[("ps", b2)])
            P.add("dve", lambda e, b1=b1: e.tensor_copy(out=small[:, 8:16], in_=ps[b1][:, 0:8]), reads=[("ps", b1)], writes=["raw"])
            P.add("dve", lambda e, b2=b2: e.tensor_copy(out=small[:, 29:30], in_=ps[b2][:, 0:1]), reads=[("ps", b2)], writes=["rcol"])
            P.add("dve", lambda e: e.max(out=small[:, 16:24], in_=small[:, 8:16]), reads=["raw"], writes=["top8"])
            P.add("dve", lambda e: e.tensor_tensor(out=small[:, 24:25], in0=small[:, 17:18], in1=small[:, 16:17], op=ALU.subtract),
                  reads=["top8"], writes=["diff"])
            P.add("act", lambda e: e.activation(out=small[:, 25:26], in_=small[:, 24:25], func=AF.Exp, scale=small[:, 29:30]),
                  reads=["diff", "rcol"], writes=["ex"])
            P.add("dve", lambda e: e.tensor_scalar(out=small[:, 26:27], in0=small[:, 25:26], scalar1=1.0, scalar2=None, op0=ALU.add),
                  reads=["ex"], writes=["den"])
            P.add("dve", lambda e: e.reciprocal(out=small[:, 27:28], in_=small[:, 26:27]), reads=["den"], writes=["g1"])
            P.add("dve", lambda e: e.tensor_tensor(out=small[:, 28:29], in0=small[:, 25:26], in1=small[:, 27:28], op=ALU.mult),
                  reads=["ex", "g1"], writes=["g2"])
            P.add("dve", lambda e: e.tensor_scalar(out=small[:, 32:40], in0=small[:, 8:16], scalar1=small[:, 16:17], scalar2=small[:, 27:28],
                                                   op0=ALU.is_equal, op1=ALU.mult), reads=["raw", "top8", "g1"], writes=["c1"])
            P.add("dve", lambda e: e.tensor_scalar(out=small[:, 40:48], in0=small[:, 8:16], scalar1=small[:, 17:18], scalar2=small[:, 28:29],
                                                   op0=ALU.is_equal, op1=ALU.mult), reads=["raw", "top8", "g2"], writes=["c2"])
            P.add("dve", lambda e: e.tensor_tensor(out=small[:, 40:48], in0=small[:, 40:48], in1=small[:, 32:40], op=ALU.add),
                  reads=["c1", "c2"], writes=["comb"])
            b3 = bank("all")
            P.add("pe", lambda e, b3=b3: e.transpose(out=ps[b3][0:8, 0:128], in_=small[:, 40:48], identity=identf[:, :]),
                  reads=["comb", "identf"], writes=[("ps", b3)])
            P.add("act", lambda e, b3=b3, c0=c0: e.activation(out=combT[0:8, c0 - 128:c0], in_=ps[b3][0:8, 0:128], func=AF.Copy),
                  reads=[("ps", b3)], writes=[("combT", q)])
        for ex in (range(NE) if stage >= 5 else []):
            gs = ex % 2
            for ti in range(3):
                bnk = bank("all")
                P.add("pe", lambda e, ex=ex, ti=ti, bnk=bnk: e.matmul(ps[bnk][:, 0:384], lhsT=selt[0:8, ex, :], rhs=combT[0:8, ti * 384:(ti + 1) * 384],
                                                                      start=True, stop=True),
                      reads=["selt"] + [("combT", q) for q in range(1, 10)], writes=[("ps", bnk)])
                P.add("act", lambda e, gs=gs, ti=ti, bnk=bnk: e.activation(out=gbc[:, gs, ti * 384:(ti + 1) * 384], in_=ps[bnk][:, 0:384], func=AF.Copy),
                      reads=[("ps", bnk)], writes=[("gbc", gs)])
            ffn_body(T1, mg[ex], mu[ex], md[ex], DFF_E // 128, gs)

        P.barrier()
        for (c0, c1) in T1:
            norm_stage(c0, c1, abuf[:, :, 0:416], "abuf", rstd[:, c0:c1], ("rstd", c0), None, None, None, do_xn=False)
        s_y = [P.dsem(), P.dsem()]
        outkeys = []
        for q in range(1, 10):
            c0 = q * 128
            tile0 = 128 if q < 4 else (512 if q < 7 else 896)
            sl = q % 2
            for c4 in range(4):
                for j in range(4):
                    c = c4 * 4 + j
                    P.add("dve", lambda e, c=c, j=j, c0=c0: e.scalar_tensor_tensor(out=ytmp[:, j, :], in0=h[:, c, c0:c0 + 128], scalar=c_gfin(c),
                                                                               in1=rstd[:, c0:c0 + 128], op0=ALU.mult, op1=ALU.mult),
                          reads=[("h", c, q), ("rstd", tile0), "cvec"], writes=[("ytmp", j)])
                bnk = bank("all")

                def tr(e, bnk=bnk):
                    ins = None
                    for j in range(4):
                        ins = e.transpose(out=ps[bnk][:, j * 128:(j + 1) * 128], in_=ytmp[:, j, :], identity=identf[:, :])
                    return ins
                P.add("pe", tr, reads=[("ytmp", j) for j in range(4)] + ["identf"], writes=[("ps", bnk)])
                evac(xstage[:, sl, c4 * 512:(c4 + 1) * 512], ps[bnk][:, :], reads=[("ps", bnk)], writes=[("xs", sl, c4)])
            P.add("sp", lambda e, q=q, sl=sl: e.dma_start(out=yout[(q - 1) * 128:q * 128, :], in_=xstage[:, sl, :]),
                  reads=[("xs", sl, c4) for c4 in range(4)], writes=[("yout", q)], dsem=s_y[sl])
            outkeys.append(("yout", q))
        outkeys += [k for k in ["dbg1", "dbg2", "dbg3"] if k in P.last_w]
        for l in range(2):
            outkeys += [k for k in [("npp", l), ("nps_a", l), ("nps_b", l), ("nvs", l)] if k in P.last_w]
        P.add("sp", lambda e: e.wait_ge(s_y[0].h, 0), reads=outkeys)

        P.assign()
        block = es.enter_context(nc.Block())
        P.finalize(block)
    return nc


def _band_mats(start_core):
    m = np.zeros((27, 128, 128), np.float32)
    m[0] = np.eye(128, dtype=np.float32)
    s = np.arange(128)[:, None]
    t = np.arange(128)[None, :]
    m[1] = (s <= t).astype(np.float32)
    m[2] = ((s // 8 == t // 8) & (s % 8 <= t % 8)).astype(np.float32)
    for g, w in enumerate(WINS):
        base = 3 + g * 6
        inwin = ((s <= t) & (s > t - w)).astype(np.float32)
        m[base + 0] = inwin / w - np.eye(128, dtype=np.float32)
        m[base + 1] = ((s - 128) > (t - w)).astype(np.float32) / w
        if start_core:
            cnt = np.minimum(t + 1, w).astype(np.float32)
            m[base + 2] = inwin / cnt - np.eye(128, dtype=np.float32)
        else:
            m[base + 2] = m[base + 0]
        sb, st_ = s // 8, s % 8
        tb, tt = t // 8, t % 8
        m[base + 3] = ((sb == tb) & (st_ <= tt) & (st_ > tt - w)).astype(np.float32) / w - np.eye(128, dtype=np.float32)
        for j in range(2):
            rows = np.arange(128)[:, None]
            rb = rows // 15 + 8 * j
            rr = rows % 15
            ok = (rows < 120) & (rb == tb) & (rr > 15 + tt - w)
            m[base + 4 + j] = ok.astype(np.float32) / w
    return np.ascontiguousarray(m.transpose(1, 0, 2))


_NC_CACHE = {}


def kernel(x_prompt, x_sample, state_pool, g_mix, w_in, g_v, w_pool, pool_scale, w_s, b_s, w_out, g_ffn,
           dense_w_gate, dense_w_up, dense_w_down, w_router, moe_w_gate, moe_w_up, moe_w_down, g_final):
    f = lambda a: np.ascontiguousarray(np.asarray(a, dtype=np.float32))
    x_prompt, x_sample, state_pool = f(x_prompt), f(x_sample), f(state_pool)
    w_s, b_s = f(w_s), f(b_s)
    if "nc" not in _NC_CACHE:
        _NC_CACHE["nc"] = build_program()
    nc = _NC_CACHE["nc"]

    def pp(v):
        return np.asarray(v, np.float32).reshape(-1, 128).T

    cvec = np.concatenate([pp(g_mix[0]), pp(g_mix[1]), pp(pool_scale[0]), pp(pool_scale[1]),
                           pp(g_ffn[0]), pp(g_ffn[1]), pp(g_final)], axis=1)
    cvec = np.ascontiguousarray(cvec, dtype=np.float32)
    gvbc = np.ascontiguousarray(np.broadcast_to(np.asarray(g_v, np.float32)[:, None, :], (2, 128, DP)))
    wsT = np.ascontiguousarray(w_s.transpose(0, 3, 1, 2))
    blk = w_s[:, :, :8, :8].transpose(0, 3, 1, 2)
    wsbig = np.ascontiguousarray(np.tile(blk, (1, 16, 1, 16)))
    bsrow = np.zeros((2, 1, 2048), np.float32)
    bsrow[:, 0, :1024] = b_s.reshape(2, 1024)
    bsrow[:, 0, 1024:] = np.tile(b_s[:, :, None, :8], (1, 1, 16, 1)).reshape(2, 1024)
    identf = np.eye(128, dtype=np.float32)
    sel = np.zeros((8, 8, 128), np.float32)
    for e in range(8):
        sel[e, e, :] = 1.0
    sel = sel.reshape(8, 1024)
    shared = dict(w_in=f(w_in), w_out=f(w_out), w_pool=f(w_pool), dg=f(dense_w_gate)[0], du=f(dense_w_up)[0], dd=f(dense_w_down)[0],
                  mg=f(moe_w_gate)[0], mu=f(moe_w_up)[0], md=f(moe_w_down)[0], wr=f(w_router)[0], wsT=wsT, wsbig=wsbig,
                  cvec=cvec, gvbc=gvbc, bsrow=bsrow, identf=identf, sel=sel)
    cm = [_band_mats(True), _band_mats(False)]
    in_maps = []
    for c in range(NCORES):
        b, half = c // 2, c % 2
        s0 = half * 1024
        xin = np.zeros((TT, D), np.float32)
        if half == 1:
            xin[0:128] = x_prompt[b, s0 - 128:s0]
        xin[128:1152] = x_prompt[b, s0:s0 + 1024]
        xin[1152:1280] = x_sample[16 * c:16 * c + 16].reshape(128, D)
        m = dict(shared)
        m["xin"] = xin
        m["spool"] = np.ascontiguousarray(state_pool[:, 16 * c:16 * c + 16].reshape(2, 240, DP))
        m["cmat"] = cm[0] if half == 0 else cm[1]
        in_maps.append(m)
    res = run_bass_kernel_spmd(nc, in_maps, core_ids=list(range(NCORES)))
    R = res.results
    y_prompt = np.zeros((4, 2048, D), np.float32)
    y_sample = np.zeros((128, 8, D), np.float32)
    npp = np.zeros((2, 4, 15, DP), np.float32)
    nps = np.zeros((2, 128, 15, DP), np.float32)
    nvs = np.zeros((2, 128, 8, DP), np.float32)
    for c in range(NCORES):
        b, half = c // 2, c % 2
        yo = np.asarray(R[c]["yout"])
        y_prompt[b, half * 1024:(half + 1) * 1024] = yo[:1024]
        y_sample[16 * c:16 * c + 16] = yo[1024:].reshape(16, 8, D)
        if half == 1:
            npp[:, b] = np.asarray(R[c]["npp"])
        nps[:, 16 * c:16 * c + 16, 0:7] = np.asarray(R[c]["nps_a"])
        nps[:, 16 * c:16 * c + 16, 7:15] = np.asarray(R[c]["nps_b"]).reshape(2, 16, 8, DP)
        nvs[:, 16 * c:16 * c + 16] = np.asarray(R[c]["nvs"]).reshape(2, 16, 8, DP)
    return (y_prompt, y_sample, npp, nps, nvs)
```

```python
import contextlib
import os
import numpy as np
import concourse.bass as bass
import concourse.mybir as mybir
from concourse.bass_utils import run_bass_kernel_spmd

F32, BF16 = mybir.dt.float32, mybir.dt.bfloat16
AF = mybir.ActivationFunctionType
ALU = mybir.AluOpType

D = 2048
KC = 16
TT = 1280
DP = 1024
DIN = 3072
DFF_D = 5632
DFF_E = 7168
NE = 8
EPS = 1e-6
NCORES = 8
WINS = (2, 4, 8, 16)
NSLOT = 5
ENGS = {"pe": "tensor", "act": "scalar", "dve": "vector", "pool": "gpsimd", "sp": "sync"}


class Op:
    __slots__ = ("eng", "fn", "deps", "signal", "dsem", "sem", "val", "inc")

    def __init__(self, eng, fn, dsem):
        self.eng, self.fn, self.dsem = eng, fn, dsem
        self.deps, self.signal, self.sem, self.val, self.inc = [], False, None, 0, 1


class DSem:
    def __init__(self, h):
        self.h, self.count = h, 0


class Prog:
    def __init__(self, nc, es):
        self.nc, self.es = nc, es
        self.ops, self.last_w, self.readers = [], {}, {}
        self.nsem = 0
        self.phase_op = None
        self.phase_idx = 0
        self.bar_tile = None

    def new_sem(self):
        self.nsem += 1
        return self.es.enter_context(self.nc.semaphore("s%d" % self.nsem))

    def dsem(self):
        return DSem(self.new_sem())

    def add(self, eng, fn, reads=(), writes=(), dsem=None):
        op = Op(eng, fn, dsem)
        deps = []
        seen = set()

        def dep(o):
            if o is None or id(o) in seen:
                return
            seen.add(id(o))
            if o.eng == "pe" and eng == "pe" and o.dsem is None:
                return
            deps.append(o)
            o.signal = True

        dep(self.phase_op)
        for k in reads:
            dep(self.last_w.get(k))
        for k in writes:
            dep(self.last_w.get(k))
            for r in self.readers.get(k, ()):
                dep(r)
        op.deps = deps
        for k in reads:
            self.readers.setdefault(k, []).append(op)
        for k in writes:
            self.last_w[k] = op
            self.readers[k] = []
        self.ops.append(op)
        return op

    def barrier(self):
        deps, seen_eng = [], set()
        for o in reversed(self.ops[self.phase_idx:]):
            if o.dsem is not None:
                deps.append(o)
            elif o.eng not in seen_eng:
                seen_eng.add(o.eng)
                deps.append(o)
        bt = self.bar_tile
        op = Op("dve", lambda e: e.memset(bt[:, :], 0.0), None)
        if self.phase_op is not None:
            deps.append(self.phase_op)
        for d in deps:
            d.signal = True
        op.deps = deps
        self.ops.append(op)
        self.phase_op = op
        self.phase_idx = len(self.ops)

    def assign(self):
        cnt, cur = {}, {}
        for op in self.ops:
            if not op.signal:
                continue
            if op.dsem is not None:
                op.dsem.count += 16
                op.sem, op.val, op.inc = op.dsem.h, op.dsem.count, 16
            else:
                e = op.eng
                if e not in cur or cnt[e] >= 30000:
                    cur[e], cnt[e] = self.new_sem(), 0
                cnt[e] += 1
                op.sem, op.val, op.inc = cur[e], cnt[e], 1

    def finalize(self, block):
        per = {e: [] for e in ENGS}
        for op in self.ops:
            per[op.eng].append(op)
        for e, lst in per.items():
            def body(eng, lst=lst):
                waited = {}
                for op in lst:
                    need = {}
                    for d in op.deps:
                        k = id(d.sem)
                        if need.get(k, (None, 0))[1] < d.val:
                            need[k] = (d.sem, d.val)
                    for k, (sm, v) in need.items():
                        if waited.get(k, 0) >= v:
                            continue
                        eng.wait_ge(sm, v)
                        waited[k] = v
                    ins = op.fn(eng)
                    if op.signal:
                        ins.then_inc(op.sem, op.inc)
            getattr(block, ENGS[e])(body)


def build_program(stage=9):
    nc = bass.Bass("TRN2", target_bir_lowering=False)

    def din(name, shape):
        return nc.dram_tensor(name, list(shape), F32, kind="ExternalInput").ap()

    def dout(name, shape):
        return nc.dram_tensor(name, list(shape), F32, kind="ExternalOutput").ap()

    xin = din("xin", [TT, D])
    spool = din("spool", [2, 240, DP])
    w_in = din("w_in", [2, D, DIN])
    w_out = din("w_out", [2, D, D])
    w_pool = din("w_pool", [2, 4, 256, 256])
    dg = din("dg", [D, DFF_D])
    du = din("du", [D, DFF_D])
    dd = din("dd", [DFF_D, D])
    if stage >= 5:
        mg = din("mg", [NE, D, DFF_E])
        mu = din("mu", [NE, D, DFF_E])
        md = din("md", [NE, DFF_E, D])
    wr = din("wr", [D, NE])
    wsT = din("wsT", [2, 128, 8, 128])
    wsbig = din("wsbig", [2, 128, 8, 128])
    cvec_d = din("cvec", [128, 96])
    gvbc_d = din("gvbc", [2, 128, DP])
    bsrow_d = din("bsrow", [2, 1, 2048])
    cmat_d = din("cmat", [128, 27, 128])
    identf_d = din("identf", [128, 128])
    sel_d = din("sel", [8, 1024])
    yout = dout("yout", [1152, D])
    npp = dout("npp", [2, 15, DP])
    nps_a = dout("nps_a", [2, 16, 7, DP])
    nps_b = dout("nps_b", [2, 128, DP])
    nvs = dout("nvs", [2, 128, DP])
    DBG = os.environ.get("DBG_DUMP")
    if DBG:
        dbg1 = nc.dram_tensor("dbg1", [128, 4096], BF16, kind="ExternalOutput").ap()
        dbg2 = dout("dbg2", [128, 256])

    with contextlib.ExitStack() as es:
        def sb(name, shape, dt):
            return es.enter_context(nc.sbuf_tensor("sb_" + name, list(shape), dt))

        P = Prog(nc, es)
        h = sb("h", [128, KC, TT], F32)
        ring = [sb("ring%d" % i, [128, 4096], BF16) for i in range(NSLOT)]
        ovl = sb("ovl", [128, 12800], F32)
        ovl2 = sb("ovl2", [128, 8192], F32)
        cvec = sb("cvec", [128, 96], F32)
        identf = sb("identf", [128, 128], F32)
        onesf = sb("onesf", [1, 128], F32)
        onesb = sb("onesb", [128, 128], BF16)
        epsc = sb("epsc", [128, 1], F32)
        small = sb("small", [128, 64], F32)
        P.bar_tile = sb("bart", [128, 2], F32)
        ps = [es.enter_context(nc.psum_tensor("ps%d" % i, [128, 512], F32)) for i in range(8)]

        def view(reg, off, dt, n):
            w = 4 if dt == F32 else 2
            a = reg[:, off // 4:(off + n * w) // 4]
            return a if dt == F32 else a.bitcast(BF16)

        xn = view(ovl, 0, BF16, KC * TT).rearrange("p (k n) -> p k n", k=KC)
        abuf = view(ovl, 40960, BF16, 4 * TT).rearrange("p (k n) -> p k n", k=4)
        xstage = view(ovl, 0, F32, 2 * D).rearrange("p (b n) -> p b n", b=2)
        ytmp = view(ovl, 16384, F32, 512).rearrange("p (j n) -> p j n", j=4)
        xn_t = view(ovl, 0, BF16, KC * 256).rearrange("p (k n) -> p k n", k=KC)
        pbf = view(ovl, 8192, BF16, 3 * DP).rearrange("p (q n) -> p q n", q=3)
        pf32 = view(ovl, 14336, F32, 2 * DP).rearrange("p (q n) -> p q n", q=2)
        vg = view(ovl, 22528, F32, DP)
        vnbf = view(ovl, 26624, BF16, 2 * DP).rearrange("p (q n) -> p q n", q=2)
        ubuf = view(ovl, 30720, BF16, 8 * 256).rearrange("p (k n) -> p k n", k=8)
        rbuf = view(ovl, 34816, BF16, 8 * 256).rearrange("p (k n) -> p k n", k=8)
        cat = view(ovl, 38912, BF16, KC * 256).rearrange("p (k n) -> p k n", k=KC)
        junk = view(ovl, 47104, BF16, DP)
        sq4m = junk.rearrange("p (k n) -> p k n", k=4)
        cmat = view(ovl2, 0, BF16, 27 * 128).rearrange("p (m n) -> p m n", m=27)
        wsTm = view(ovl2, 6912, BF16, 1024).rearrange("p (h n) -> p h n", h=8)
        wsbm = view(ovl2, 8960, BF16, 1024).rearrange("p (h n) -> p h n", h=8)
        gvbc = view(ovl2, 11008, F32, DP)
        wpool = view(ovl2, 15104, BF16, 2048).rearrange("p (g k n) -> p g k n", g=4, k=2)
        bsrow = view(ovl2, 19200, F32, 2048)
        spre = view(ovl2, 27392, BF16, 2 * DP).rearrange("p (j n) -> p j n", j=2)
        rstd = view(ovl2, 0, F32, TT)
        selt = view(ovl2, 5120, F32, 1024).rearrange("p (e n) -> p e n", e=8)
        combT = view(ovl2, 9216, F32, 1152)
        gbc = view(ovl2, 13824, F32, 2 * 1152).rearrange("p (b n) -> p b n", b=2)
        wrg = view(ovl2, 23040, F32, KC * 8).rearrange("p (k e) -> p k e", k=KC)
        silt = view(ovl2, 23552, BF16, 2 * 416).rearrange("p (b n) -> p b n", b=2)
        tmp2 = view(ovl2, 25216, F32, 416)
        rstd_m = view(ovl2, 31488, F32, 256)

        def c_gmix(l, k): return cvec[:, l * 16 + k:l * 16 + k + 1]
        def c_psc(l, k): return cvec[:, 32 + l * 8 + k:32 + l * 8 + k + 1]
        def c_gffn(l, k): return cvec[:, 48 + l * 16 + k:48 + l * 16 + k + 1]
        def c_gfin(k): return cvec[:, 80 + k:80 + k + 1]

        rot = {"lo": 0, "hi": 0, "all": 0}

        def bank(which):
            if which == "hi":
                b = 4 + rot["hi"] % 4
                rot["hi"] += 1
            else:
                b = rot["all"] % 8
                rot["all"] += 1
            return b

        rstate = {"i": 0}
        rsems = [P.dsem() for _ in range(NSLOT)]

        def ring_load(src3d, shape3, mdl=4096):
            s = rstate["i"] % NSLOT
            rstate["i"] += 1
            dst = ring[s][:, :].rearrange("p (a b) -> p a b", a=shape3[0])
            P.add("pool", lambda e: e.dma_start(out=dst, in_=src3d, max_dma_last_dim=mdl), writes=[("ring", s)], dsem=rsems[s])
            return s, dst

        def wblock(w2d, c0):
            return ring_load(w2d.rearrange("(k p) n -> p k n", p=128)[:, :, c0:c0 + 256], (16, 256))

        def dblock(w2d, r0):
            return ring_load(w2d[r0:r0 + 256, :].rearrange("(k p) n -> p k n", p=128), (2, 2048), 8192)

        s_c = P.dsem()
        P.add("sp", lambda e: e.dma_start(out=cvec[:, :], in_=cvec_d[:, :]), writes=["cvec"], dsem=s_c)
        s_i = P.dsem()
        P.add("sp", lambda e: e.dma_start(out=identf[:, :], in_=identf_d[:, :]), writes=["identf"], dsem=s_i)
        P.add("dve", lambda e: e.memset(onesf[:, :], 1.0), writes=["onesf"])
        P.add("dve", lambda e: e.memset(onesb[:, :], 1.0), writes=["onesb"])
        P.add("dve", lambda e: e.memset(epsc[:, :], EPS), writes=["epsc"])

        s_x = [P.dsem(), P.dsem()]
        flip = [0]

        def evac(out_ap, in_ap, reads, writes):
            flip[0] ^= 1
            if flip[0]:
                P.add("act", lambda e: e.activation(out=out_ap, in_=in_ap, func=AF.Copy), reads=reads, writes=writes)
            else:
                P.add("dve", lambda e: e.tensor_copy(out=out_ap, in_=in_ap), reads=reads, writes=writes)

        for q in range(10):
            sl = q % 2
            P.add("sp", lambda e, q=q, sl=sl: e.dma_start(out=xstage[:, sl, :], in_=xin[q * 128:(q + 1) * 128, :]),
                  writes=[("xs", sl)], dsem=s_x[sl])
            for c4 in range(4):
                b = bank("all")

                def tr(e, b=b, sl=sl, c4=c4):
                    ins = None
                    for j in range(4):
                        c = c4 * 4 + j
                        ins = e.transpose(out=ps[b][:, j * 128:(j + 1) * 128], in_=xstage[:, sl, c * 128:(c + 1) * 128],
                                          identity=identf[:, :])
                    return ins
                P.add("pe", tr, reads=[("xs", sl), "identf"], writes=[("ps", b)])
                evac(h[:, c4 * 4:c4 * 4 + 4, q * 128:(q + 1) * 128], ps[b][:, :].rearrange("p (j n) -> p j n", j=4),
                     reads=[("ps", b)], writes=[("h", c4 * 4 + j, q) for j in range(4)])

        def hkeys(k, c0, c1):
            return [("h", k, q) for q in range(c0 // 128, (c1 + 127) // 128)]

        def norm_stage(c0, c1, sq4, sqkey, rs, rskey, gcol, xn_out, xnkey, do_xn=True):
            W = c1 - c0
            b = bank("hi")
            for kk in range(4):
                P.add("act", lambda e, kk=kk: e.activation(out=sq4[:, :, 0:W], in_=h[:, kk * 4:kk * 4 + 4, c0:c1], func=AF.Square),
                      reads=sum([hkeys(kk * 4 + j, c0, c1) for j in range(4)], []), writes=[sqkey])

                def mm(e, kk=kk):
                    ins = None
                    for j in range(4):
                        ins = e.matmul(ps[b][:, 0:W], lhsT=onesb[:, :], rhs=sq4[:, j, 0:W],
                                       start=(kk == 0 and j == 0), stop=(kk == 3 and j == 3))
                    return ins
                P.add("pe", mm, reads=[sqkey, "onesb"], writes=[("ps", b)])
            P.add("act", lambda e: e.activation(out=rs[:, 0:W], in_=ps[b][:, 0:W], func=AF.Sqrt, bias=epsc[:, 0:1], scale=1.0 / D),
                  reads=[("ps", b), "epsc"], writes=[rskey])
            P.add("dve", lambda e: e.reciprocal(out=rs[:, 0:W], in_=rs[:, 0:W]), reads=[rskey], writes=[rskey])
            if do_xn:
                for k in range(KC):
                    P.add("dve", lambda e, k=k: e.scalar_tensor_tensor(out=xn_out[:, k, 0:W], in0=h[:, k, c0:c1], scalar=gcol(k),
                                                                    in1=rs[:, 0:W], op0=ALU.mult, op1=ALU.mult),
                          reads=hkeys(k, c0, c1) + [rskey, "cvec"], writes=[(xnkey, k)])

        s_l = [P.dsem() for _ in range(7)]
        s_o = [P.dsem() for _ in range(4)]

        def mixer(l):
            SM = int(os.environ.get("SETUP_MASK", "511"))
            if SM & 1:
                P.add("pool", lambda e: e.dma_start(out=cmat, in_=cmat_d[:, :, :], max_dma_last_dim=4096), writes=["cmat"], dsem=s_l[0])
            if SM & 2:
                P.add("pool", lambda e: e.dma_start(out=wsTm, in_=wsT[l], max_dma_last_dim=4096), writes=["wsTm"], dsem=s_l[1])
            if SM & 4:
                P.add("pool", lambda e: e.dma_start(out=wsbm, in_=wsbig[l], max_dma_last_dim=4096), writes=["wsbm"], dsem=s_l[2])
            if SM & 8:
                P.add("pool", lambda e: e.dma_start(out=wpool, in_=w_pool[l].rearrange("g (k p) n -> p g k n", p=128), max_dma_last_dim=4096),
                      writes=["wpool"], dsem=s_l[3])
            if SM & 16:
                P.add("pool", lambda e: e.dma_start(out=spre[0:120, :, :], in_=spool[l].rearrange("(j r) n -> r j n", j=2), max_dma_last_dim=4096),
                      writes=["spre"], dsem=s_l[4])
            if SM & 32:
                P.add("sp", lambda e: e.dma_start(out=gvbc, in_=gvbc_d[l]), writes=["gvbc"], dsem=s_l[5])
            if SM & 64:
                P.add("sp", lambda e: e.dma_start(out=bsrow[0:1, :], in_=bsrow_d[l]), writes=["bsrow"], dsem=s_l[6])
            for hh in (range(8) if SM & 128 else []):
                P.add("dve", lambda e, hh=hh: e.tensor_tensor(out=wsTm[:, hh, :], in0=wsTm[:, hh, :], in1=cmat[:, 1, :], op=ALU.mult),
                      reads=["wsTm", "cmat"], writes=["wsTm"])
                P.add("dve", lambda e, hh=hh: e.tensor_tensor(out=wsbm[:, hh, :], in0=wsbm[:, hh, :], in1=cmat[:, 2, :], op=ALU.mult),
                      reads=["wsbm", "cmat"], writes=["wsbm"])
            if SM & 256:
                P.add("sp", lambda e: e.dma_start(out=nps_a[l], in_=spool[l].rearrange("(b r) n -> b r n", r=15)[:, 8:15, :]),
                      writes=[("nps_a", l)], dsem=s_o[0])

            MS = int(os.environ.get("MIX_STEPS", "99"))
            for ti in range(int(os.environ.get("MIX_TILES", "5"))):
                c0 = ti * 256
                qa = 2 * ti
                if MS < 1:
                    continue
                norm_stage(c0, c0 + 256, sq4m, "junk", rstd_m, "rstd_m", lambda k: c_gmix(l, k), xn_t, "xn_t")
                xnk = [("xn_t", k) for k in range(KC)]

                def tokmajor(cbase):
                    for bq in range(4):
                        s, wv = wblock(w_in[l], cbase + bq * 256)
                        for qi in range(2):
                            bnk = 2 * qi + bq // 2

                            def mm(e, wv=wv, qi=qi, bnk=bnk, bq=bq):
                                ins = None
                                for k in range(KC):
                                    ins = e.matmul(ps[bnk][:, (bq % 2) * 256:(bq % 2) * 256 + 256], lhsT=xn_t[:, k, qi * 128:(qi + 1) * 128],
                                                   rhs=wv[:, k, :], start=(k == 0), stop=(k == KC - 1))
                                return ins
                            P.add("pe", mm, reads=xnk + [("ring", s)], writes=[("ps", bnk)])

                if DBG and ti == 4 and l == 0:
                    sd1, sd2 = P.dsem(), P.dsem()
                    P.add("sp", lambda e: e.dma_start(out=dbg1, in_=xn_t.rearrange("p k n -> p (k n)")), reads=xnk, writes=["dbg1"], dsem=sd1)
                    P.add("sp", lambda e: e.dma_start(out=dbg2, in_=rstd_m), reads=["rstd_m"], writes=["dbg2"], dsem=sd2)
                if MS < 2:
                    continue
                tokmajor(0)
                for qi in range(2):
                    q = qa + qi
                    for hf in range(2):
                        bnk = 2 * qi + hf
                        P.add("act", lambda e, q=q, hf=hf, bnk=bnk: e.activation(out=pbf[:, q % 3, hf * 512:(hf + 1) * 512], in_=ps[bnk][:, :], func=AF.Copy),
                              reads=[("ps", bnk)], writes=[("pbf", q % 3)])
                        if q >= 8 and not os.environ.get("NO_PF32"):
                            P.add("act", lambda e, q=q, hf=hf, bnk=bnk: e.activation(out=pf32[:, q - 8, hf * 512:(hf + 1) * 512], in_=ps[bnk][:, :], func=AF.Copy),
                                  reads=[("ps", bnk)], writes=[("pf32", q - 8)])
                    if q == 8 and not os.environ.get("NO_NPP"):
                        P.add("sp", lambda e: e.dma_start(out=npp[l], in_=pf32[113:128, 0, :]), reads=[("pf32", 0)], writes=[("npp", l)], dsem=s_o[1])
                    if q == 9 and not os.environ.get("NO_NPSB"):
                        P.add("sp", lambda e: e.dma_start(out=nps_b[l], in_=pf32[:, 1, :]), reads=[("pf32", 1)], writes=[("nps_b", l)], dsem=s_o[2])
                if DBG and ti == int(os.environ.get("DBG_TI", "0")) and l == 0:
                    sd3 = P.dsem()
                    dbg3 = nc.dram_tensor("dbg3", [128, 3072], BF16, kind="ExternalOutput").ap()
                    P.add("sp", lambda e: e.dma_start(out=dbg3, in_=pbf.rearrange("p q n -> p (q n)")), reads=[("pbf", i) for i in range(3)], writes=["dbg3"], dsem=sd3)
                if MS < 3:
                    continue
                for bq in range(4):
                    s, wv = wblock(w_in[l], DP + bq * 256)
                    for m in range(2):
                        bnk = bank("hi")

                        def mm(e, wv=wv, m=m, bnk=bnk):
                            ins = None
                            for k in range(KC):
                                ins = e.matmul(ps[bnk][:, 0:256], lhsT=wv[:, k, m * 128:(m + 1) * 128], rhs=xn_t[:, k, :],
                                               start=(k == 0), stop=(k == KC - 1))
                            return ins
                        P.add("pe", mm, reads=xnk + [("ring", s)], writes=[("ps", bnk)])
                        P.add("act", lambda e, bq=bq, m=m, bnk=bnk: e.activation(out=ubuf[:, bq * 2 + m, :], in_=ps[bnk][:, 0:256], func=AF.Gelu),
                              reads=[("ps", bnk)], writes=[("u", bq * 2 + m)])
                if MS < 4:
                    continue
                tokmajor(2 * DP)
                for qi in range(2):
                    q = qa + qi
                    for hf in range(2):
                        bnk = 2 * qi + hf
                        P.add("act", lambda e, hf=hf, bnk=bnk: e.activation(out=vg[:, hf * 512:(hf + 1) * 512], in_=ps[bnk][:, :], func=AF.Gelu),
                              reads=[("ps", bnk)], writes=["vg"])
                    P.add("dve", lambda e: e.scalar_tensor_tensor(out=junk, in0=vg, scalar=1.0, in1=vg, op0=ALU.mult, op1=ALU.mult,
                                                                  accum_out=small[:, 0:1]),
                          reads=["vg"], writes=["junk", "ss"])
                    P.add("act", lambda e: e.activation(out=small[:, 1:2], in_=small[:, 0:1], func=AF.Sqrt, bias=epsc[:, 0:1], scale=1.0 / DP),
                          reads=["ss", "epsc"], writes=["ss1"])
                    P.add("dve", lambda e: e.reciprocal(out=small[:, 2:3], in_=small[:, 1:2]), reads=["ss1"], writes=["ss2"])
                    if q < 9:
                        P.add("dve", lambda e, q=q: e.scalar_tensor_tensor(out=vnbf[:, q % 2, :], in0=vg, scalar=small[:, 2:3], in1=gvbc,
                                                                        op0=ALU.mult, op1=ALU.mult),
                              reads=["vg", "ss2", "gvbc"], writes=[("vnbf", q % 2)])
                    else:
                        P.add("dve", lambda e: e.scalar_tensor_tensor(out=vg, in0=vg, scalar=small[:, 2:3], in1=gvbc, op0=ALU.mult, op1=ALU.mult),
                              reads=["vg", "ss2", "gvbc"], writes=["vg"])
                        P.add("act", lambda e, q=q: e.activation(out=vnbf[:, q % 2, :], in_=vg, func=AF.Copy), reads=["vg"], writes=[("vnbf", q % 2)])
                        P.add("sp", lambda e: e.dma_start(out=nvs[l], in_=vg), reads=["vg"], writes=[("nvs", l)], dsem=s_o[3])

                if MS < 5:
                    continue
                for qi in range(2):
                    q = qa + qi
                    for half in range(2):
                        bnk = bank("hi")

                        def mm(e, q=q, half=half, bnk=bnk):
                            ins = None
                            for j in range(4):
                                fc = half * 4 + j
                                g = fc // 2
                                o = ps[bnk][:, j * 128:(j + 1) * 128]
                                lt = pbf[:, q % 3, fc * 128:(fc + 1) * 128]
                                if q == 9:
                                    e.matmul(o, lhsT=lt, rhs=cmat[:, 3 + g * 6 + 3, :], start=True, stop=False)
                                    e.matmul(o, lhsT=spre[0:120, 0, fc * 128:(fc + 1) * 128], rhs=cmat[0:120, 3 + g * 6 + 4, :], start=False, stop=False)
                                    ins = e.matmul(o, lhsT=spre[0:120, 1, fc * 128:(fc + 1) * 128], rhs=cmat[0:120, 3 + g * 6 + 5, :], start=False, stop=True)
                                elif q == 0:
                                    ins = e.matmul(o, lhsT=lt, rhs=cmat[:, 3 + g * 6 + 0, :], start=True, stop=True)
                                else:
                                    e.matmul(o, lhsT=lt, rhs=cmat[:, 3 + g * 6 + (2 if q == 1 else 0), :], start=True, stop=False)
                                    ins = e.matmul(o, lhsT=pbf[:, (q - 1) % 3, fc * 128:(fc + 1) * 128], rhs=cmat[:, 3 + g * 6 + 1, :],
                                                   start=False, stop=True)
                            return ins
                        rd = [("pbf", q % 3), "cmat"] + ([("pbf", (q - 1) % 3)] if 1 <= q <= 8 else []) + (["spre"] if q == 9 else [])
                        P.add("pe", mm, reads=rd, writes=[("ps", bnk)])
                        evac(rbuf[:, half * 4:half * 4 + 4, qi * 128:(qi + 1) * 128], ps[bnk][:, :].rearrange("p (j n) -> p j n", j=4),
                             reads=[("ps", bnk)], writes=[("r", half * 4 + j, qi) for j in range(4)])
                for fo in range(8):
                    g, m = fo // 2, fo % 2
                    bnk = bank("hi")

                    def mm(e, g=g, m=m, bnk=bnk):
                        e.matmul(ps[bnk][:, 0:256], lhsT=wpool[:, g, 0, m * 128:(m + 1) * 128], rhs=rbuf[:, 2 * g, :], start=True, stop=False)
                        return e.matmul(ps[bnk][:, 0:256], lhsT=wpool[:, g, 1, m * 128:(m + 1) * 128], rhs=rbuf[:, 2 * g + 1, :], start=False, stop=True)
                    P.add("pe", mm, reads=["wpool"] + [("r", 2 * g + kk, qi) for kk in range(2) for qi in range(2)], writes=[("ps", bnk)])
                    P.add("act", lambda e, fo=fo, bnk=bnk: e.activation(out=cat[:, fo, :], in_=ps[bnk][:, 0:256], func=AF.Copy, scale=c_psc(l, fo)),
                          reads=[("ps", bnk), "cvec"], writes=[("cat", fo)])
                if MS < 6:
                    continue
                for qi in range(2):
                    q = qa + qi
                    for hh in (0, 4):
                        bnk = bank("hi")

                        def mm(e, q=q, hh=hh, bnk=bnk):
                            ins = None
                            for j in range(4):
                                hd = hh + j
                                o = ps[bnk][:, j * 128:(j + 1) * 128]
                                wm = wsbm if q == 9 else wsTm
                                boff = (1024 if q == 9 else 0) + hd * 128
                                e.matmul(o, lhsT=vnbf[:, q % 2, hd * 128:(hd + 1) * 128], rhs=wm[:, hd, :], start=True, stop=False)
                                ins = e.matmul(o, lhsT=onesf[0:1, :], rhs=bsrow[0:1, boff:boff + 128], start=False, stop=True)
                            return ins
                        P.add("pe", mm, reads=[("vnbf", q % 2), "wsTm", "wsbm", "bsrow", "onesf"], writes=[("ps", bnk)])
                        P.add("dve", lambda e, hh=hh, qi=qi, bnk=bnk: e.tensor_tensor(
                            out=cat[:, 8 + hh:12 + hh, qi * 128:(qi + 1) * 128], in0=ubuf[:, hh:hh + 4, qi * 128:(qi + 1) * 128],
                            in1=ps[bnk][:, :].rearrange("p (j n) -> p j n", j=4), op=ALU.mult),
                            reads=[("ps", bnk)] + [("u", hh + j) for j in range(4)], writes=[("cat", 8 + hh + j) for j in range(4)])
                if MS < 7:
                    continue
                catk = [("cat", k) for k in range(KC)]
                for bq in range(8):
                    s, wv = wblock(w_out[l], bq * 256)
                    for m in range(2):
                        bnk = bank("hi")
                        dc = bq * 2 + m

                        def mm(e, wv=wv, m=m, bnk=bnk):
                            ins = None
                            for k in range(KC):
                                ins = e.matmul(ps[bnk][:, 0:256], lhsT=wv[:, k, m * 128:(m + 1) * 128], rhs=cat[:, k, :],
                                               start=(k == 0), stop=(k == KC - 1))
                            return ins
                        P.add("pe", mm, reads=catk + [("ring", s)], writes=[("ps", bnk)])
                        P.add("dve", lambda e, dc=dc, bnk=bnk, c0=c0: e.tensor_tensor(out=h[:, dc, c0:c0 + 256], in0=h[:, dc, c0:c0 + 256],
                                                                             in1=ps[bnk][:, 0:256], op=ALU.add),
                              reads=[("ps", bnk)] + hkeys(dc, c0, c0 + 256), writes=hkeys(dc, c0, c0 + 256))

        def ffn_norm(l, tiles):
            for (c0, c1) in tiles:
                sq4 = abuf[:, :, 0:416]
                norm_stage(c0, c1, sq4, "abuf", rstd[:, c0:c1], ("rstd", c0), lambda k: c_gffn(l, k), xn[:, :, c0:c1], ("xn", c0))

        def ffn_body(tiles, wg2d, wu2d, wd2d, nchunks, gsel):
            for gi in range(nchunks // 4):
                f0 = gi * 4
                for hb in range(2):
                    sg, wgv = wblock(wg2d, (f0 + hb * 2) * 128)
                    su, wuv = wblock(wu2d, (f0 + hb * 2) * 128)
                    for fi2 in range(2):
                        fi = hb * 2 + fi2
                        for ti, (c0, c1) in enumerate(tiles):
                            W = c1 - c0
                            bg, bu = bank("all"), bank("all")
                            xk = [(("xn", c0), k) for k in range(KC)]

                            def mmg(e, wgv=wgv, fi2=fi2, bg=bg, c0=c0, c1=c1, W=W):
                                ins = None
                                for k in range(KC):
                                    ins = e.matmul(ps[bg][:, 0:W], lhsT=wgv[:, k, fi2 * 128:(fi2 + 1) * 128], rhs=xn[:, k, c0:c1],
                                                   start=(k == 0), stop=(k == KC - 1))
                                return ins

                            def mmu(e, wuv=wuv, fi2=fi2, bu=bu, c0=c0, c1=c1, W=W):
                                ins = None
                                for k in range(KC):
                                    ins = e.matmul(ps[bu][:, 0:W], lhsT=wuv[:, k, fi2 * 128:(fi2 + 1) * 128], rhs=xn[:, k, c0:c1],
                                                   start=(k == 0), stop=(k == KC - 1))
                                return ins
                            P.add("pe", mmg, reads=xk + [("ring", sg)], writes=[("ps", bg)])
                            P.add("pe", mmu, reads=xk + [("ring", su)], writes=[("ps", bu)])
                            sb_ = (fi * 3 + ti) % 2
                            P.add("act", lambda e, bg=bg, W=W, sb_=sb_: e.activation(out=silt[:, sb_, 0:W], in_=ps[bg][:, 0:W], func=AF.Silu),
                                  reads=[("ps", bg)], writes=[("silt", sb_)])
                            if gsel is None:
                                P.add("dve", lambda e, bu=bu, W=W, sb_=sb_, fi=fi, c0=c0, c1=c1: e.tensor_tensor(
                                    out=abuf[:, fi, c0:c1], in0=silt[:, sb_, 0:W], in1=ps[bu][:, 0:W], op=ALU.mult),
                                    reads=[("ps", bu), ("silt", sb_)], writes=[("a", fi, c0)])
                            else:
                                P.add("dve", lambda e, bu=bu, W=W, sb_=sb_: e.tensor_tensor(
                                    out=tmp2[:, 0:W], in0=silt[:, sb_, 0:W], in1=ps[bu][:, 0:W], op=ALU.mult),
                                    reads=[("ps", bu), ("silt", sb_)], writes=["tmp2"])
                                P.add("dve", lambda e, W=W, fi=fi, c0=c0, c1=c1: e.tensor_tensor(
                                    out=abuf[:, fi, c0:c1], in0=tmp2[:, 0:W], in1=gbc[:, gsel, c0 - 128:c1 - 128], op=ALU.mult),
                                    reads=["tmp2", ("gbc", gsel)], writes=[("a", fi, c0)])
                sd = [dblock(wd2d, (f0 + hb * 2) * 128) for hb in range(2)]
                for (c0, c1) in tiles:
                    for dc in range(KC):
                        W = c1 - c0
                        bnk = bank("all")

                        def mmd(e, dc=dc, bnk=bnk, c0=c0, c1=c1, W=W, sd=sd):
                            ins = None
                            for fi in range(4):
                                ins = e.matmul(ps[bnk][:, 0:W], lhsT=sd[fi // 2][1][:, fi % 2, dc * 128:(dc + 1) * 128], rhs=abuf[:, fi, c0:c1],
                                               start=(fi == 0), stop=(fi == 3))
                            return ins
                        P.add("pe", mmd, reads=[("a", fi, c0) for fi in range(4)] + [("ring", sd[0][0]), ("ring", sd[1][0])], writes=[("ps", bnk)])
                        P.add("dve", lambda e, dc=dc, bnk=bnk, c0=c0, c1=c1, W=W: e.tensor_tensor(
                            out=h[:, dc, c0:c1], in0=h[:, dc, c0:c1], in1=ps[bnk][:, 0:W], op=ALU.add),
                            reads=[("ps", bnk)] + hkeys(dc, c0, c1), writes=hkeys(dc, c0, c1))

        T0 = [(96, 512), (512, 896), (896, 1280)]
        T1 = [(128, 512), (512, 896), (896, 1280)]

        P.barrier()
        if stage >= 1:
            mixer(0)
        P.barrier()
        if stage >= 2:
            ffn_norm(0, T0)
            ffn_body(T0, dg, du, dd, DFF_D // 128, None)
        P.barrier()
        if stage >= 3:
            mixer(1)
        P.barrier()
        ffn_norm(1, T1)
        s_r = [P.dsem(), P.dsem()]
        P.add("sp", lambda e: e.dma_start(out=wrg, in_=wr.rearrange("(k p) e -> p k e", p=128)), writes=["wrg"], dsem=s_r[0])
        P.add("sp", lambda e: e.dma_start(out=selt[0:8, :, :], in_=sel_d.rearrange("k (e n) -> k e n", e=8)), writes=["selt"], dsem=s_r[1])
        for k in range(KC):
            P.add("dve", lambda e, k=k: e.tensor_scalar(out=wrg[:, k, :], in0=wrg[:, k, :], scalar1=c_gffn(1, k), scalar2=None, op0=ALU.mult),
                  reads=["wrg", "cvec"], writes=["wrg"])
        for q in (range(1, 10) if stage >= 4 else []):
            c0 = q * 128
            tile0 = 128 if q < 4 else (512 if q < 7 else 896)
            b1 = bank("all")

            def mmr(e, b1=b1, c0=c0):
                ins = None
                for k in range(KC):
                    ins = e.matmul(ps[b1][:, 0:8], lhsT=h[:, k, c0:c0 + 128], rhs=wrg[:, k, :], start=(k == 0), stop=(k == KC - 1))
                return ins
            P.add("pe", mmr, reads=["wrg"] + [("h", k, q) for k in range(KC)], writes=[("ps", b1)])
            b2 = bank("all")
            P.add("pe", lambda e, b2=b2, c0=c0: e.matmul(ps[b2][:, 0:1], lhsT=rstd[0:1, c0:c0 + 128], rhs=onesf[0:1, 0:1], start=True, stop=True),
                  reads=[("rstd", tile0), "onesf"], writes=[("ps", b2)])
            P.add("dve", lambda e, b1=b1: e.tensor_copy(out=small[:, 8:16], in_=ps[b1][:, 0:8]), reads=[("ps", b1)], writes=["raw"])
            P.add("dve", lambda e, b2=b2: e.tensor_copy(out=small[:, 29:30], in_=ps[b2][:, 0:1]), reads=[("ps", b2)], writes=["rcol"])
            P.add("dve", lambda e: e.max(out=small[:, 16:24], in_=small[:, 8:16]), reads=["raw"], writes=["top8"])
            P.add("dve", lambda e: e.tensor_tensor(out=small[:, 24:25], in0=small[:, 17:18], in1=small[:, 16:17], op=ALU.subtract),
                  reads=["top8"], writes=["diff"])
            P.add("act", lambda e: e.activation(out=small[:, 25:26], in_=small[:, 24:25], func=AF.Exp, scale=small[:, 29:30]),
                  reads=["diff", "rcol"], writes=["ex"])
            P.add("dve", lambda e: e.tensor_scalar(out=small[:, 26:27], in0=small[:, 25:26], scalar1=1.0, scalar2=None, op0=ALU.add),
                  reads=["ex"], writes=["den"])
            P.add("dve", lambda e: e.reciprocal(out=small[:, 27:28], in_=small[:, 26:27]), reads=["den"], writes=["g1"])
            P.add("dve", lambda e: e.tensor_tensor(out=small[:, 28:29], in0=small[:, 25:26], in1=small[:, 27:28], op=ALU.mult),
                  reads=["ex", "g1"], writes=["g2"])
            P.add("dve", lambda e: e.tensor_scalar(out=small[:, 32:40], in0=small[:, 8:16], scalar1=small[:, 16:17], scalar2=small[:, 27:28],
                                                   op0=ALU.is_equal, op1=ALU.mult), reads=["raw", "top8", "g1"], writes=["c1"])
            P.add("dve", lambda e: e.tensor_scalar(out=small[:, 40:48], in0=small[:, 8:16], scalar1=small[:, 17:18], scalar2=small[:, 28:29],
                                                   op0=ALU.is_equal, op1=ALU.mult), reads=["raw", "top8", "g2"], writes=["c2"])
            P.add("dve", lambda e: e.tensor_tensor(out=small[:, 40:48], in0=small[:, 40:48], in1=small[:, 32:40], op=ALU.add),
                  reads=["c1", "c2"], writes=["comb"])
            b3 = bank("all")
            P.add("pe", lambda e, b3=b3: e.transpose(out=ps[b3][0:8, 0:128], in_=small[:, 40:48], identity=identf[:, :]),
                  reads=["comb", "identf"], writes=[("ps", b3)])
            P.add("act", lambda e, b3=b3, c0=c0: e.activation(out=combT[0:8, c0 - 128:c0], in_=ps[b3][0:8, 0:128], func=AF.Copy),
                  reads=[("ps", b3)], writes=[("combT", q)])
        for ex in (range(NE) if stage >= 5 else []):
            gs = ex % 2
            for ti in range(3):
                bnk = bank("all")
                P.add("pe", lambda e, ex=ex, ti=ti, bnk=bnk: e.matmul(ps[bnk][:, 0:384], lhsT=selt[0:8, ex, :], rhs=combT[0:8, ti * 384:(ti + 1) * 384],
                                                                      start=True, stop=True),
                      reads=["selt"] + [("combT", q) for q in range(1, 10)], writes=[("ps", bnk)])
                P.add("act", lambda e, gs=gs, ti=ti, bnk=bnk: e.activation(out=gbc[:, gs, ti * 384:(ti + 1) * 384], in_=ps[bnk][:, 0:384], func=AF.Copy),
                      reads=[("ps", bnk)], writes=[("gbc", gs)])
            ffn_body(T1, mg[ex], mu[ex], md[ex], DFF_E // 128, gs)

        P.barrier()
        for (c0, c1) in T1:
            norm_stage(c0, c1, abuf[:, :, 0:416], "abuf", rstd[:, c0:c1], ("rstd", c0), None, None, None, do_xn=False)
        s_y = [P.dsem(), P.dsem()]
        outkeys = []
        for q in range(1, 10):
            c0 = q * 128
            tile0 = 128 if q < 4 else (512 if q < 7 else 896)
            sl = q % 2
            for c4 in range(4):
                for j in range(4):
                    c = c4 * 4 + j
                    P.add("dve", lambda e, c=c, j=j, c0=c0: e.scalar_tensor_tensor(out=ytmp[:, j, :], in0=h[:, c, c0:c0 + 128], scalar=c_gfin(c),
                                                                               in1=rstd[:, c0:c0 + 128], op0=ALU.mult, op1=ALU.mult),
                          reads=[("h", c, q), ("rstd", tile0), "cvec"], writes=[("ytmp", j)])
                bnk = bank("all")

                def tr(e, bnk=bnk):
                    ins = None
                    for j in range(4):
                        ins = e.transpose(out=ps[bnk][:, j * 128:(j + 1) * 128], in_=ytmp[:, j, :], identity=identf[:, :])
                    return ins
                P.add("pe", tr, reads=[("ytmp", j) for j in range(4)] + ["identf"], writes=[("ps", bnk)])
                evac(xstage[:, sl, c4 * 512:(c4 + 1) * 512], ps[bnk][:, :], reads=[("ps", bnk)], writes=[("xs", sl, c4)])
            P.add("sp", lambda e, q=q, sl=sl: e.dma_start(out=yout[(q - 1) * 128:q * 128, :], in_=xstage[:, sl, :]),
                  reads=[("xs", sl, c4) for c4 in range(4)], writes=[("yout", q)], dsem=s_y[sl])
            outkeys.append(("yout", q))
        outkeys += [k for k in ["dbg1", "dbg2", "dbg3"] if k in P.last_w]
        for l in range(2):
            outkeys += [k for k in [("npp", l), ("nps_a", l), ("nps_b", l), ("nvs", l)] if k in P.last_w]
        P.add("sp", lambda e: e.wait_ge(s_y[0].h, 0), reads=outkeys)

        P.assign()
        block = es.enter_context(nc.Block())
        P.finalize(block)
    return nc


def _band_mats(start_core):
    m = np.zeros((27, 128, 128), np.float32)
    m[0] = np.eye(128, dtype=np.float32)
    s = np.arange(128)[:, None]
    t = np.arange(128)[None, :]
    m[1] = (s <= t).astype(np.float32)
    m[2] = ((s // 8 == t // 8) & (s % 8 <= t % 8)).astype(np.float32)
    for g, w in enumerate(WINS):
        base = 3 + g * 6
        inwin = ((s <= t) & (s > t - w)).astype(np.float32)
        m[base + 0] = inwin / w - np.eye(128, dtype=np.float32)
        m[base + 1] = ((s - 128) > (t - w)).astype(np.float32) / w
        if start_core:
            cnt = np.minimum(t + 1, w).astype(np.float32)
            m[base + 2] = inwin / cnt - np.eye(128, dtype=np.float32)
        else:
            m[base + 2] = m[base + 0]
        sb, st_ = s // 8, s % 8
        tb, tt = t // 8, t % 8
        m[base + 3] = ((sb == tb) & (st_ <= tt) & (st_ > tt - w)).astype(np.float32) / w - np.eye(128, dtype=np.float32)
        for j in range(2):
            rows = np.arange(128)[:, None]
            rb = rows // 15 + 8 * j
            rr = rows % 15
            ok = (rows < 120) & (rb == tb) & (rr > 15 + tt - w)
            m[base + 4 + j] = ok.astype(np.float32) / w
    return np.ascontiguousarray(m.transpose(1, 0, 2))


_NC_CACHE = {}


def kernel(x_prompt, x_sample, state_pool, g_mix, w_in, g_v, w_pool, pool_scale, w_s, b_s, w_out, g_ffn,
           dense_w_gate, dense_w_up, dense_w_down, w_router, moe_w_gate, moe_w_up, moe_w_down, g_final):
    f = lambda a: np.ascontiguousarray(np.asarray(a, dtype=np.float32))
    x_prompt, x_sample, state_pool = f(x_prompt), f(x_sample), f(state_pool)
    w_s, b_s = f(w_s), f(b_s)
    if "nc" not in _NC_CACHE:
        _NC_CACHE["nc"] = build_program()
    nc = _NC_CACHE["nc"]

    def pp(v):
        return np.asarray(v, np.float32).reshape(-1, 128).T

    cvec = np.concatenate([pp(g_mix[0]), pp(g_mix[1]), pp(pool_scale[0]), pp(pool_scale[1]),
                           pp(g_ffn[0]), pp(g_ffn[1]), pp(g_final)], axis=1)
    cvec = np.ascontiguousarray(cvec, dtype=np.float32)
    gvbc = np.ascontiguousarray(np.broadcast_to(np.asarray(g_v, np.float32)[:, None, :], (2, 128, DP)))
    wsT = np.ascontiguousarray(w_s.transpose(0, 3, 1, 2))
    blk = w_s[:, :, :8, :8].transpose(0, 3, 1, 2)
    wsbig = np.ascontiguousarray(np.tile(blk, (1, 16, 1, 16)))
    bsrow = np.zeros((2, 1, 2048), np.float32)
    bsrow[:, 0, :1024] = b_s.reshape(2, 1024)
    bsrow[:, 0, 1024:] = np.tile(b_s[:, :, None, :8], (1, 1, 16, 1)).reshape(2, 1024)
    identf = np.eye(128, dtype=np.float32)
    sel = np.zeros((8, 8, 128), np.float32)
    for e in range(8):
        sel[e, e, :] = 1.0
    sel = sel.reshape(8, 1024)
    shared = dict(w_in=f(w_in), w_out=f(w_out), w_pool=f(w_pool), dg=f(dense_w_gate)[0], du=f(dense_w_up)[0], dd=f(dense_w_down)[0],
                  mg=f(moe_w_gate)[0], mu=f(moe_w_up)[0], md=f(moe_w_down)[0], wr=f(w_router)[0], wsT=wsT, wsbig=wsbig,
                  cvec=cvec, gvbc=gvbc, bsrow=bsrow, identf=identf, sel=sel)
    cm = [_band_mats(True), _band_mats(False)]
    in_maps = []
    for c in range(NCORES):
        b, half = c // 2, c % 2
        s0 = half * 1024
        xin = np.zeros((TT, D), np.float32)
        if half == 1:
            xin[0:128] = x_prompt[b, s0 - 128:s0]
        xin[128:1152] = x_prompt[b, s0:s0 + 1024]
        xin[1152:1280] = x_sample[16 * c:16 * c + 16].reshape(128, D)
        m = dict(shared)
        m["xin"] = xin
        m["spool"] = np.ascontiguousarray(state_pool[:, 16 * c:16 * c + 16].reshape(2, 240, DP))
        m["cmat"] = cm[0] if half == 0 else cm[1]
        in_maps.append(m)
    res = run_bass_kernel_spmd(nc, in_maps, core_ids=list(range(NCORES)))
    R = res.results
    y_prompt = np.zeros((4, 2048, D), np.float32)
    y_sample = np.zeros((128, 8, D), np.float32)
    npp = np.zeros((2, 4, 15, DP), np.float32)
    nps = np.zeros((2, 128, 15, DP), np.float32)
    nvs = np.zeros((2, 128, 8, DP), np.float32)
    for c in range(NCORES):
        b, half = c // 2, c % 2
        yo = np.asarray(R[c]["yout"])
        y_prompt[b, half * 1024:(half + 1) * 1024] = yo[:1024]
        y_sample[16 * c:16 * c + 16] = yo[1024:].reshape(16, 8, D)
        if half == 1:
            npp[:, b] = np.asarray(R[c]["npp"])
        nps[:, 16 * c:16 * c + 16, 0:7] = np.asarray(R[c]["nps_a"])
        nps[:, 16 * c:16 * c + 16, 7:15] = np.asarray(R[c]["nps_b"]).reshape(2, 16, 8, DP)
        nvs[:, 16 * c:16 * c + 16] = np.asarray(R[c]["nvs"]).reshape(2, 16, 8, DP)
    return (y_prompt, y_sample, npp, nps, nvs)
```

```python
import contextlib
import numpy as np
import concourse.bass as bass
import concourse.mybir as mybir
from concourse.bass_utils import run_bass_kernel_spmd

F32, BF16 = mybir.dt.float32, mybir.dt.bfloat16
AF = mybir.ActivationFunctionType
ALU = mybir.AluOpType

D = 2048
KC = 16
TT = 1280
DP = 1024
DIN = 3072
DFF_D = 5632
DFF_E = 7168
NE = 8
EPS = 1e-6
NCORES = 8
WINS = (2, 4, 8, 16)
NSLOT = 5
_DBG = {}
ENGS = {"pe": "tensor", "act": "scalar", "dve": "vector", "pool": "gpsimd", "sp": "sync"}


class Op:
    __slots__ = ("eng", "fn", "deps", "signal", "dsem", "sem", "val", "inc")

    def __init__(self, eng, fn, dsem):
        self.eng, self.fn, self.dsem = eng, fn, dsem
        self.deps, self.signal, self.sem, self.val, self.inc = [], False, None, 0, 1


class DSem:
    def __init__(self, h):
        self.h, self.count = h, 0


class Prog:
    def __init__(self, nc, es):
        self.nc, self.es = nc, es
        self.ops, self.last_w, self.readers = [], {}, {}
        self.nsem = 0
        self.phase_op = None
        self.phase_idx = 0
        self.bar_tile = None

    def new_sem(self):
        self.nsem += 1
        return self.es.enter_context(self.nc.semaphore("s%d" % self.nsem))

    def dsem(self):
        return DSem(self.new_sem())

    def add(self, eng, fn, reads=(), writes=(), dsem=None):
        op = Op(eng, fn, dsem)
        deps = []
        seen = set()

        def dep(o):
            if o is None or id(o) in seen:
                return
            seen.add(id(o))
            if o.eng == "pe" and eng == "pe" and o.dsem is None:
                return
            deps.append(o)
            o.signal = True

        dep(self.phase_op)
        for k in reads:
            dep(self.last_w.get(k))
        for k in writes:
            dep(self.last_w.get(k))
            for r in self.readers.get(k, ()):
                dep(r)
        op.deps = deps
        for k in reads:
            self.readers.setdefault(k, []).append(op)
        for k in writes:
            self.last_w[k] = op
            self.readers[k] = []
        self.ops.append(op)
        return op

    def barrier(self):
        deps, seen_eng = [], set()
        for o in reversed(self.ops[self.phase_idx:]):
            if o.dsem is not None:
                deps.append(o)
            elif o.eng not in seen_eng:
                seen_eng.add(o.eng)
                deps.append(o)
        bt = self.bar_tile
        op = Op("dve", lambda e: e.memset(bt[:, :], 0.0), None)
        if self.phase_op is not None:
            deps.append(self.phase_op)
        for d in deps:
            d.signal = True
        op.deps = deps
        self.ops.append(op)
        self.phase_op = op
        self.phase_idx = len(self.ops)

    def assign(self):
        cnt, cur = {}, {}
        for op in self.ops:
            if not op.signal:
                continue
            if op.dsem is not None:
                op.dsem.count += 16
                op.sem, op.val, op.inc = op.dsem.h, op.dsem.count, 16
            else:
                e = op.eng
                if e not in cur or cnt[e] >= 30000:
                    cur[e], cnt[e] = self.new_sem(), 0
                cnt[e] += 1
                op.sem, op.val, op.inc = cur[e], cnt[e], 1

    def finalize(self, block):
        per = {e: [] for e in ENGS}
        for op in self.ops:
            per[op.eng].append(op)
        for e, lst in per.items():
            def body(eng, lst=lst):
                waited = {}
                for op in lst:
                    need = {}
                    for d in op.deps:
                        k = id(d.sem)
                        if need.get(k, (None, 0))[1] < d.val:
                            need[k] = (d.sem, d.val)
                    for k, (sm, v) in need.items():
                        if waited.get(k, 0) >= v:
                            continue
                        eng.wait_ge(sm, v)
                        waited[k] = v
                    ins = op.fn(eng)
                    if op.signal:
                        ins.then_inc(op.sem, op.inc)
            getattr(block, ENGS[e])(body)


def build_program(stage=9):
    nc = bass.Bass("TRN2", target_bir_lowering=False)

    def din(name, shape):
        return nc.dram_tensor(name, list(shape), F32, kind="ExternalInput").ap()

    def dout(name, shape):
        return nc.dram_tensor(name, list(shape), F32, kind="ExternalOutput").ap()

    xin = din("xin", [TT, D])
    spool = din("spool", [2, 240, DP])
    w_in = din("w_in", [2, D, DIN])
    w_out = din("w_out", [2, D, D])
    w_pool = din("w_pool", [2, 4, 256, 256])
    dg = din("dg", [D, DFF_D])
    du = din("du", [D, DFF_D])
    dd = din("dd", [DFF_D, D])
    if stage >= 5:
        mg = din("mg", [NE, D, DFF_E])
        mu = din("mu", [NE, D, DFF_E])
        md = din("md", [NE, DFF_E, D])
    wr = din("wr", [D, NE])
    wsT = din("wsT", [2, 128, 8, 128])
    wsbig = din("wsbig", [2, 128, 8, 128])
    cvec_d = din("cvec", [128, 96])
    gvbc_d = din("gvbc", [2, 128, DP])
    bsrow_d = din("bsrow", [2, 1, 2048])
    cmat_d = din("cmat", [128, 27, 128])
    identf_d = din("identf", [128, 128])
    sel_d = din("sel", [8, 1024])
    yout = dout("yout", [1152, D])
    npp = dout("npp", [2, 15, DP])
    nps_a = dout("nps_a", [2, 16, 7, DP])
    nps_b = dout("nps_b", [2, 128, DP])
    nvs = dout("nvs", [2, 128, DP])
    DBG = _DBG.get("DBG_DUMP")
    if DBG:
        dbg1 = nc.dram_tensor("dbg1", [128, 4096], BF16, kind="ExternalOutput").ap()
        dbg2 = dout("dbg2", [128, 256])

    with contextlib.ExitStack() as es:
        def sb(name, shape, dt):
            return es.enter_context(nc.sbuf_tensor("sb_" + name, list(shape), dt))

        P = Prog(nc, es)
        h = sb("h", [128, KC, TT], F32)
        ring = [sb("ring%d" % i, [128, 4096], BF16) for i in range(NSLOT)]
        ovl = sb("ovl", [128, 12800], F32)
        ovl2 = sb("ovl2", [128, 8192], F32)
        cvec = sb("cvec", [128, 96], F32)
        identf = sb("identf", [128, 128], F32)
        onesf = sb("onesf", [1, 128], F32)
        onesb = sb("onesb", [128, 128], BF16)
        epsc = sb("epsc", [128, 1], F32)
        small = sb("small", [128, 64], F32)
        P.bar_tile = sb("bart", [128, 2], F32)
        ps = [es.enter_context(nc.psum_tensor("ps%d" % i, [128, 512], F32)) for i in range(8)]

        def view(reg, off, dt, n):
            w = 4 if dt == F32 else 2
            a = reg[:, off // 4:(off + n * w) // 4]
            return a if dt == F32 else a.bitcast(BF16)

        xn = view(ovl, 0, BF16, KC * TT).rearrange("p (k n) -> p k n", k=KC)
        abuf = view(ovl, 40960, BF16, 4 * TT).rearrange("p (k n) -> p k n", k=4)
        xstage = view(ovl, 0, F32, 2 * D).rearrange("p (b n) -> p b n", b=2)
        ytmp = view(ovl, 16384, F32, 512).rearrange("p (j n) -> p j n", j=4)
        xn_t = view(ovl, 0, BF16, KC * 256).rearrange("p (k n) -> p k n", k=KC)
        pbf = view(ovl, 8192, BF16, 3 * DP).rearrange("p (q n) -> p q n", q=3)
        pf32 = view(ovl, 14336, F32, 2 * DP).rearrange("p (q n) -> p q n", q=2)
        vg = view(ovl, 22528, F32, DP)
        vnbf = view(ovl, 26624, BF16, 2 * DP).rearrange("p (q n) -> p q n", q=2)
        ubuf = view(ovl, 30720, BF16, 8 * 256).rearrange("p (k n) -> p k n", k=8)
        rbuf = view(ovl, 34816, BF16, 8 * 256).rearrange("p (k n) -> p k n", k=8)
        cat = view(ovl, 38912, BF16, KC * 256).rearrange("p (k n) -> p k n", k=KC)
        junk = view(ovl, 47104, BF16, DP)
        sq4m = junk.rearrange("p (k n) -> p k n", k=4)
        cmat = view(ovl2, 0, BF16, 27 * 128).rearrange("p (m n) -> p m n", m=27)
        wsTm = view(ovl2, 6912, BF16, 1024).rearrange("p (h n) -> p h n", h=8)
        wsbm = view(ovl2, 8960, BF16, 1024).rearrange("p (h n) -> p h n", h=8)
        gvbc = view(ovl2, 11008, F32, DP)
        wpool = view(ovl2, 15104, BF16, 2048).rearrange("p (g k n) -> p g k n", g=4, k=2)
        bsrow = view(ovl2, 19200, F32, 2048)
        spre = view(ovl2, 27392, BF16, 2 * DP).rearrange("p (j n) -> p j n", j=2)
        rstd = view(ovl2, 0, F32, TT)
        selt = view(ovl2, 5120, F32, 1024).rearrange("p (e n) -> p e n", e=8)
        combT = view(ovl2, 9216, F32, 1152)
        gbc = view(ovl2, 13824, F32, 2 * 1152).rearrange("p (b n) -> p b n", b=2)
        wrg = view(ovl2, 23040, F32, KC * 8).rearrange("p (k e) -> p k e", k=KC)
        silt = view(ovl2, 23552, BF16, 2 * 416).rearrange("p (b n) -> p b n", b=2)
        tmp2 = view(ovl2, 25216, F32, 416)
        rstd_m = view(ovl2, 31488, F32, 256)

        def c_gmix(l, k): return cvec[:, l * 16 + k:l * 16 + k + 1]
        def c_psc(l, k): return cvec[:, 32 + l * 8 + k:32 + l * 8 + k + 1]
        def c_gffn(l, k): return cvec[:, 48 + l * 16 + k:48 + l * 16 + k + 1]
        def c_gfin(k): return cvec[:, 80 + k:80 + k + 1]

        rot = {"lo": 0, "hi": 0, "all": 0}

        def bank(which):
            if which == "hi":
                b = 4 + rot["hi"] % 4
                rot["hi"] += 1
            else:
                b = rot["all"] % 8
                rot["all"] += 1
            return b

        rstate = {"i": 0}
        rsems = [P.dsem() for _ in range(NSLOT)]

        def ring_load(src3d, shape3, mdl=4096):
            s = rstate["i"] % NSLOT
            rstate["i"] += 1
            dst = ring[s][:, :].rearrange("p (a b) -> p a b", a=shape3[0])
            P.add("pool", lambda e: e.dma_start(out=dst, in_=src3d, max_dma_last_dim=mdl), writes=[("ring", s)], dsem=rsems[s])
            return s, dst

        def wblock(w2d, c0):
            return ring_load(w2d.rearrange("(k p) n -> p k n", p=128)[:, :, c0:c0 + 256], (16, 256))

        def dblock(w2d, r0):
            return ring_load(w2d[r0:r0 + 256, :].rearrange("(k p) n -> p k n", p=128), (2, 2048), 8192)

        s_c = P.dsem()
        P.add("sp", lambda e: e.dma_start(out=cvec[:, :], in_=cvec_d[:, :]), writes=["cvec"], dsem=s_c)
        s_i = P.dsem()
        P.add("sp", lambda e: e.dma_start(out=identf[:, :], in_=identf_d[:, :]), writes=["identf"], dsem=s_i)
        P.add("dve", lambda e: e.memset(onesf[:, :], 1.0), writes=["onesf"])
        P.add("dve", lambda e: e.memset(onesb[:, :], 1.0), writes=["onesb"])
        P.add("dve", lambda e: e.memset(epsc[:, :], EPS), writes=["epsc"])

        s_x = [P.dsem(), P.dsem()]
        flip = [0]

        def evac(out_ap, in_ap, reads, writes):
            flip[0] ^= 1
            if flip[0]:
                P.add("act", lambda e: e.activation(out=out_ap, in_=in_ap, func=AF.Copy), reads=reads, writes=writes)
            else:
                P.add("dve", lambda e: e.tensor_copy(out=out_ap, in_=in_ap), reads=reads, writes=writes)

        for q in range(10):
            sl = q % 2
            P.add("sp", lambda e, q=q, sl=sl: e.dma_start(out=xstage[:, sl, :], in_=xin[q * 128:(q + 1) * 128, :]),
                  writes=[("xs", sl)], dsem=s_x[sl])
            for c4 in range(4):
                b = bank("all")

                def tr(e, b=b, sl=sl, c4=c4):
                    ins = None
                    for j in range(4):
                        c = c4 * 4 + j
                        ins = e.transpose(out=ps[b][:, j * 128:(j + 1) * 128], in_=xstage[:, sl, c * 128:(c + 1) * 128],
                                          identity=identf[:, :])
                    return ins
                P.add("pe", tr, reads=[("xs", sl), "identf"], writes=[("ps", b)])
                evac(h[:, c4 * 4:c4 * 4 + 4, q * 128:(q + 1) * 128], ps[b][:, :].rearrange("p (j n) -> p j n", j=4),
                     reads=[("ps", b)], writes=[("h", c4 * 4 + j, q) for j in range(4)])

        def hkeys(k, c0, c1):
            return [("h", k, q) for q in range(c0 // 128, (c1 + 127) // 128)]

        def norm_stage(c0, c1, sq4, sqkey, rs, rskey, gcol, xn_out, xnkey, do_xn=True):
            W = c1 - c0
            b = bank("hi")
            for kk in range(4):
                P.add("act", lambda e, kk=kk: e.activation(out=sq4[:, :, 0:W], in_=h[:, kk * 4:kk * 4 + 4, c0:c1], func=AF.Square),
                      reads=sum([hkeys(kk * 4 + j, c0, c1) for j in range(4)], []), writes=[sqkey])

                def mm(e, kk=kk):
                    ins = None
                    for j in range(4):
                        ins = e.matmul(ps[b][:, 0:W], lhsT=onesb[:, :], rhs=sq4[:, j, 0:W],
                                       start=(kk == 0 and j == 0), stop=(kk == 3 and j == 3))
                    return ins
                P.add("pe", mm, reads=[sqkey, "onesb"], writes=[("ps", b)])
            P.add("act", lambda e: e.activation(out=rs[:, 0:W], in_=ps[b][:, 0:W], func=AF.Sqrt, bias=epsc[:, 0:1], scale=1.0 / D),
                  reads=[("ps", b), "epsc"], writes=[rskey])
            P.add("dve", lambda e: e.reciprocal(out=rs[:, 0:W], in_=rs[:, 0:W]), reads=[rskey], writes=[rskey])
            if do_xn:
                for k in range(KC):
                    P.add("dve", lambda e, k=k: e.scalar_tensor_tensor(out=xn_out[:, k, 0:W], in0=h[:, k, c0:c1], scalar=gcol(k),
                                                                    in1=rs[:, 0:W], op0=ALU.mult, op1=ALU.mult),
                          reads=hkeys(k, c0, c1) + [rskey, "cvec"], writes=[(xnkey, k)])

        s_l = [P.dsem() for _ in range(7)]
        s_o = [P.dsem() for _ in range(4)]

        def mixer(l):
            SM = int(_DBG.get("SETUP_MASK", "511"))
            if SM & 1:
                P.add("pool", lambda e: e.dma_start(out=cmat, in_=cmat_d[:, :, :], max_dma_last_dim=4096), writes=["cmat"], dsem=s_l[0])
            if SM & 2:
                P.add("pool", lambda e: e.dma_start(out=wsTm, in_=wsT[l], max_dma_last_dim=4096), writes=["wsTm"], dsem=s_l[1])
            if SM & 4:
                P.add("pool", lambda e: e.dma_start(out=wsbm, in_=wsbig[l], max_dma_last_dim=4096), writes=["wsbm"], dsem=s_l[2])
            if SM & 8:
                P.add("pool", lambda e: e.dma_start(out=wpool, in_=w_pool[l].rearrange("g (k p) n -> p g k n", p=128), max_dma_last_dim=4096),
                      writes=["wpool"], dsem=s_l[3])
            if SM & 16:
                P.add("pool", lambda e: e.dma_start(out=spre[0:120, :, :], in_=spool[l].rearrange("(j r) n -> r j n", j=2), max_dma_last_dim=4096),
                      writes=["spre"], dsem=s_l[4])
            if SM & 32:
                P.add("sp", lambda e: e.dma_start(out=gvbc, in_=gvbc_d[l]), writes=["gvbc"], dsem=s_l[5])
            if SM & 64:
                P.add("sp", lambda e: e.dma_start(out=bsrow[0:1, :], in_=bsrow_d[l]), writes=["bsrow"], dsem=s_l[6])
            for hh in (range(8) if SM & 128 else []):
                P.add("dve", lambda e, hh=hh: e.tensor_tensor(out=wsTm[:, hh, :], in0=wsTm[:, hh, :], in1=cmat[:, 1, :], op=ALU.mult),
                      reads=["wsTm", "cmat"], writes=["wsTm"])
                P.add("dve", lambda e, hh=hh: e.tensor_tensor(out=wsbm[:, hh, :], in0=wsbm[:, hh, :], in1=cmat[:, 2, :], op=ALU.mult),
                      reads=["wsbm", "cmat"], writes=["wsbm"])
            if SM & 256:
                P.add("sp", lambda e: e.dma_start(out=nps_a[l], in_=spool[l].rearrange("(b r) n -> b r n", r=15)[:, 8:15, :]),
                      writes=[("nps_a", l)], dsem=s_o[0])

            MS = int(_DBG.get("MIX_STEPS", "99"))
            for ti in range(int(_DBG.get("MIX_TILES", "5"))):
                c0 = ti * 256
                qa = 2 * ti
                if MS < 1:
                    continue
                norm_stage(c0, c0 + 256, sq4m, "junk", rstd_m, "rstd_m", lambda k: c_gmix(l, k), xn_t, "xn_t")
                xnk = [("xn_t", k) for k in range(KC)]

                def tokmajor(cbase):
                    for bq in range(4):
                        s, wv = wblock(w_in[l], cbase + bq * 256)
                        for qi in range(2):
                            bnk = 2 * qi + bq // 2

                            def mm(e, wv=wv, qi=qi, bnk=bnk, bq=bq):
                                ins = None
                                for k in range(KC):
                                    ins = e.matmul(ps[bnk][:, (bq % 2) * 256:(bq % 2) * 256 + 256], lhsT=xn_t[:, k, qi * 128:(qi + 1) * 128],
                                                   rhs=wv[:, k, :], start=(k == 0), stop=(k == KC - 1))
                                return ins
                            P.add("pe", mm, reads=xnk + [("ring", s)], writes=[("ps", bnk)])

                if DBG and ti == 4 and l == 0:
                    sd1, sd2 = P.dsem(), P.dsem()
                    P.add("sp", lambda e: e.dma_start(out=dbg1, in_=xn_t.rearrange("p k n -> p (k n)")), reads=xnk, writes=["dbg1"], dsem=sd1)
                    P.add("sp", lambda e: e.dma_start(out=dbg2, in_=rstd_m), reads=["rstd_m"], writes=["dbg2"], dsem=sd2)
                if MS < 2:
                    continue
                tokmajor(0)
                for qi in range(2):
                    q = qa + qi
                    for hf in range(2):
                        bnk = 2 * qi + hf
                        P.add("act", lambda e, q=q, hf=hf, bnk=bnk: e.activation(out=pbf[:, q % 3, hf * 512:(hf + 1) * 512], in_=ps[bnk][:, :], func=AF.Copy),
                              reads=[("ps", bnk)], writes=[("pbf", q % 3)])
                        if q >= 8 and not _DBG.get("NO_PF32"):
                            P.add("act", lambda e, q=q, hf=hf, bnk=bnk: e.activation(out=pf32[:, q - 8, hf * 512:(hf + 1) * 512], in_=ps[bnk][:, :], func=AF.Copy),
                                  reads=[("ps", bnk)], writes=[("pf32", q - 8)])
                    if q == 8 and not _DBG.get("NO_NPP"):
                        P.add("sp", lambda e: e.dma_start(out=npp[l], in_=pf32[113:128, 0, :]), reads=[("pf32", 0)], writes=[("npp", l)], dsem=s_o[1])
                    if q == 9 and not _DBG.get("NO_NPSB"):
                        P.add("sp", lambda e: e.dma_start(out=nps_b[l], in_=pf32[:, 1, :]), reads=[("pf32", 1)], writes=[("nps_b", l)], dsem=s_o[2])
                if DBG and ti == int(_DBG.get("DBG_TI", "0")) and l == 0:
                    sd3 = P.dsem()
                    dbg3 = nc.dram_tensor("dbg3", [128, 3072], BF16, kind="ExternalOutput").ap()
                    P.add("sp", lambda e: e.dma_start(out=dbg3, in_=pbf.rearrange("p q n -> p (q n)")), reads=[("pbf", i) for i in range(3)], writes=["dbg3"], dsem=sd3)
                if MS < 3:
                    continue
                for bq in range(4):
                    s, wv = wblock(w_in[l], DP + bq * 256)
                    for m in range(2):
                        bnk = bank("hi")

                        def mm(e, wv=wv, m=m, bnk=bnk):
                            ins = None
                            for k in range(KC):
                                ins = e.matmul(ps[bnk][:, 0:256], lhsT=wv[:, k, m * 128:(m + 1) * 128], rhs=xn_t[:, k, :],
                                               start=(k == 0), stop=(k == KC - 1))
                            return ins
                        P.add("pe", mm, reads=xnk + [("ring", s)], writes=[("ps", bnk)])
                        P.add("act", lambda e, bq=bq, m=m, bnk=bnk: e.activation(out=ubuf[:, bq * 2 + m, :], in_=ps[bnk][:, 0:256], func=AF.Gelu),
                              reads=[("ps", bnk)], writes=[("u", bq * 2 + m)])
                if MS < 4:
                    continue
                tokmajor(2 * DP)
                for qi in range(2):
                    q = qa + qi
                    for hf in range(2):
                        bnk = 2 * qi + hf
                        P.add("act", lambda e, hf=hf, bnk=bnk: e.activation(out=vg[:, hf * 512:(hf + 1) * 512], in_=ps[bnk][:, :], func=AF.Gelu),
                              reads=[("ps", bnk)], writes=["vg"])
                    P.add("dve", lambda e: e.scalar_tensor_tensor(out=junk, in0=vg, scalar=1.0, in1=vg, op0=ALU.mult, op1=ALU.mult,
                                                                  accum_out=small[:, 0:1]),
                          reads=["vg"], writes=["junk", "ss"])
                    P.add("act", lambda e: e.activation(out=small[:, 1:2], in_=small[:, 0:1], func=AF.Sqrt, bias=epsc[:, 0:1], scale=1.0 / DP),
                          reads=["ss", "epsc"], writes=["ss1"])
                    P.add("dve", lambda e: e.reciprocal(out=small[:, 2:3], in_=small[:, 1:2]), reads=["ss1"], writes=["ss2"])
                    if q < 9:
                        P.add("dve", lambda e, q=q: e.scalar_tensor_tensor(out=vnbf[:, q % 2, :], in0=vg, scalar=small[:, 2:3], in1=gvbc,
                                                                        op0=ALU.mult, op1=ALU.mult),
                              reads=["vg", "ss2", "gvbc"], writes=[("vnbf", q % 2)])
                    else:
                        P.add("dve", lambda e: e.scalar_tensor_tensor(out=vg, in0=vg, scalar=small[:, 2:3], in1=gvbc, op0=ALU.mult, op1=ALU.mult),
                              reads=["vg", "ss2", "gvbc"], writes=["vg"])
                        P.add("act", lambda e, q=q: e.activation(out=vnbf[:, q % 2, :], in_=vg, func=AF.Copy), reads=["vg"], writes=[("vnbf", q % 2)])
                        P.add("sp", lambda e: e.dma_start(out=nvs[l], in_=vg), reads=["vg"], writes=[("nvs", l)], dsem=s_o[3])

                if MS < 5:
                    continue
                for qi in range(2):
                    q = qa + qi
                    for half in range(2):
                        bnk = bank("hi")

                        def mm(e, q=q, half=half, bnk=bnk):
                            ins = None
                            for j in range(4):
                                fc = half * 4 + j
                                g = fc // 2
                                o = ps[bnk][:, j * 128:(j + 1) * 128]
                                lt = pbf[:, q % 3, fc * 128:(fc + 1) * 128]
                                if q == 9:
                                    e.matmul(o, lhsT=lt, rhs=cmat[:, 3 + g * 6 + 3, :], start=True, stop=False)
                                    e.matmul(o, lhsT=spre[0:120, 0, fc * 128:(fc + 1) * 128], rhs=cmat[0:120, 3 + g * 6 + 4, :], start=False, stop=False)
                                    ins = e.matmul(o, lhsT=spre[0:120, 1, fc * 128:(fc + 1) * 128], rhs=cmat[0:120, 3 + g * 6 + 5, :], start=False, stop=True)
                                elif q == 0:
                                    ins = e.matmul(o, lhsT=lt, rhs=cmat[:, 3 + g * 6 + 0, :], start=True, stop=True)
                                else:
                                    e.matmul(o, lhsT=lt, rhs=cmat[:, 3 + g * 6 + (2 if q == 1 else 0), :], start=True, stop=False)
                                    ins = e.matmul(o, lhsT=pbf[:, (q - 1) % 3, fc * 128:(fc + 1) * 128], rhs=cmat[:, 3 + g * 6 + 1, :],
                                                   start=False, stop=True)
                            return ins
                        rd = [("pbf", q % 3), "cmat"] + ([("pbf", (q - 1) % 3)] if 1 <= q <= 8 else []) + (["spre"] if q == 9 else [])
                        P.add("pe", mm, reads=rd, writes=[("ps", bnk)])
                        evac(rbuf[:, half * 4:half * 4 + 4, qi * 128:(qi + 1) * 128], ps[bnk][:, :].rearrange("p (j n) -> p j n", j=4),
                             reads=[("ps", bnk)], writes=[("r", half * 4 + j, qi) for j in range(4)])
                for fo in range(8):
                    g, m = fo // 2, fo % 2
                    bnk = bank("hi")

                    def mm(e, g=g, m=m, bnk=bnk):
                        e.matmul(ps[bnk][:, 0:256], lhsT=wpool[:, g, 0, m * 128:(m + 1) * 128], rhs=rbuf[:, 2 * g, :], start=True, stop=False)
                        return e.matmul(ps[bnk][:, 0:256], lhsT=wpool[:, g, 1, m * 128:(m + 1) * 128], rhs=rbuf[:, 2 * g + 1, :], start=False, stop=True)
                    P.add("pe", mm, reads=["wpool"] + [("r", 2 * g + kk, qi) for kk in range(2) for qi in range(2)], writes=[("ps", bnk)])
                    P.add("act", lambda e, fo=fo, bnk=bnk: e.activation(out=cat[:, fo, :], in_=ps[bnk][:, 0:256], func=AF.Copy, scale=c_psc(l, fo)),
                          reads=[("ps", bnk), "cvec"], writes=[("cat", fo)])
                if MS < 6:
                    continue
                for qi in range(2):
                    q = qa + qi
                    for hh in (0, 4):
                        bnk = bank("hi")

                        def mm(e, q=q, hh=hh, bnk=bnk):
                            ins = None
                            for j in range(4):
                                hd = hh + j
                                o = ps[bnk][:, j * 128:(j + 1) * 128]
                                wm = wsbm if q == 9 else wsTm
                                boff = (1024 if q == 9 else 0) + hd * 128
                                e.matmul(o, lhsT=vnbf[:, q % 2, hd * 128:(hd + 1) * 128], rhs=wm[:, hd, :], start=True, stop=False)
                                ins = e.matmul(o, lhsT=onesf[0:1, :], rhs=bsrow[0:1, boff:boff + 128], start=False, stop=True)
                            return ins
                        P.add("pe", mm, reads=[("vnbf", q % 2), "wsTm", "wsbm", "bsrow", "onesf"], writes=[("ps", bnk)])
                        P.add("dve", lambda e, hh=hh, qi=qi, bnk=bnk: e.tensor_tensor(
                            out=cat[:, 8 + hh:12 + hh, qi * 128:(qi + 1) * 128], in0=ubuf[:, hh:hh + 4, qi * 128:(qi + 1) * 128],
                            in1=ps[bnk][:, :].rearrange("p (j n) -> p j n", j=4), op=ALU.mult),
                            reads=[("ps", bnk)] + [("u", hh + j) for j in range(4)], writes=[("cat", 8 + hh + j) for j in range(4)])
                if MS < 7:
                    continue
                catk = [("cat", k) for k in range(KC)]
                for bq in range(8):
                    s, wv = wblock(w_out[l], bq * 256)
                    for m in range(2):
                        bnk = bank("hi")
                        dc = bq * 2 + m

                        def mm(e, wv=wv, m=m, bnk=bnk):
                            ins = None
                            for k in range(KC):
                                ins = e.matmul(ps[bnk][:, 0:256], lhsT=wv[:, k, m * 128:(m + 1) * 128], rhs=cat[:, k, :],
                                               start=(k == 0), stop=(k == KC - 1))
                            return ins
                        P.add("pe", mm, reads=catk + [("ring", s)], writes=[("ps", bnk)])
                        P.add("dve", lambda e, dc=dc, bnk=bnk, c0=c0: e.tensor_tensor(out=h[:, dc, c0:c0 + 256], in0=h[:, dc, c0:c0 + 256],
                                                                             in1=ps[bnk][:, 0:256], op=ALU.add),
                              reads=[("ps", bnk)] + hkeys(dc, c0, c0 + 256), writes=hkeys(dc, c0, c0 + 256))

        def ffn_norm(l, tiles):
            for (c0, c1) in tiles:
                sq4 = abuf[:, :, 0:416]
                norm_stage(c0, c1, sq4, "abuf", rstd[:, c0:c1], ("rstd", c0), lambda k: c_gffn(l, k), xn[:, :, c0:c1], ("xn", c0))

        def ffn_body(tiles, wg2d, wu2d, wd2d, nchunks, gsel):
            for gi in range(nchunks // 4):
                f0 = gi * 4
                for hb in range(2):
                    sg, wgv = wblock(wg2d, (f0 + hb * 2) * 128)
                    su, wuv = wblock(wu2d, (f0 + hb * 2) * 128)
                    for fi2 in range(2):
                        fi = hb * 2 + fi2
                        for ti, (c0, c1) in enumerate(tiles):
                            W = c1 - c0
                            bg, bu = bank("all"), bank("all")
                            xk = [(("xn", c0), k) for k in range(KC)]

                            def mmg(e, wgv=wgv, fi2=fi2, bg=bg, c0=c0, c1=c1, W=W):
                                ins = None
                                for k in range(KC):
                                    ins = e.matmul(ps[bg][:, 0:W], lhsT=wgv[:, k, fi2 * 128:(fi2 + 1) * 128], rhs=xn[:, k, c0:c1],
                                                   start=(k == 0), stop=(k == KC - 1))
                                return ins

                            def mmu(e, wuv=wuv, fi2=fi2, bu=bu, c0=c0, c1=c1, W=W):
                                ins = None
                                for k in range(KC):
                                    ins = e.matmul(ps[bu][:, 0:W], lhsT=wuv[:, k, fi2 * 128:(fi2 + 1) * 128], rhs=xn[:, k, c0:c1],
                                                   start=(k == 0), stop=(k == KC - 1))
                                return ins
                            P.add("pe", mmg, reads=xk + [("ring", sg)], writes=[("ps", bg)])
                            P.add("pe", mmu, reads=xk + [("ring", su)], writes=[("ps", bu)])
                            sb_ = (fi * 3 + ti) % 2
                            P.add("act", lambda e, bg=bg, W=W, sb_=sb_: e.activation(out=silt[:, sb_, 0:W], in_=ps[bg][:, 0:W], func=AF.Silu),
                                  reads=[("ps", bg)], writes=[("silt", sb_)])
                            if gsel is None:
                                P.add("dve", lambda e, bu=bu, W=W, sb_=sb_, fi=fi, c0=c0, c1=c1: e.tensor_tensor(
                                    out=abuf[:, fi, c0:c1], in0=silt[:, sb_, 0:W], in1=ps[bu][:, 0:W], op=ALU.mult),
                                    reads=[("ps", bu), ("silt", sb_)], writes=[("a", fi, c0)])
                            else:
                                P.add("dve", lambda e, bu=bu, W=W, sb_=sb_: e.tensor_tensor(
                                    out=tmp2[:, 0:W], in0=silt[:, sb_, 0:W], in1=ps[bu][:, 0:W], op=ALU.mult),
                                    reads=[("ps", bu), ("silt", sb_)], writes=["tmp2"])
                                P.add("dve", lambda e, W=W, fi=fi, c0=c0, c1=c1: e.tensor_tensor(
                                    out=abuf[:, fi, c0:c1], in0=tmp2[:, 0:W], in1=gbc[:, gsel, c0 - 128:c1 - 128], op=ALU.mult),
                                    reads=["tmp2", ("gbc", gsel)], writes=[("a", fi, c0)])
                sd = [dblock(wd2d, (f0 + hb * 2) * 128) for hb in range(2)]
                for (c0, c1) in tiles:
                    for dc in range(KC):
                        W = c1 - c0
                        bnk = bank("all")

                        def mmd(e, dc=dc, bnk=bnk, c0=c0, c1=c1, W=W, sd=sd):
                            ins = None
                            for fi in range(4):
                                ins = e.matmul(ps[bnk][:, 0:W], lhsT=sd[fi // 2][1][:, fi % 2, dc * 128:(dc + 1) * 128], rhs=abuf[:, fi, c0:c1],
                                               start=(fi == 0), stop=(fi == 3))
                            return ins
                        P.add("pe", mmd, reads=[("a", fi, c0) for fi in range(4)] + [("ring", sd[0][0]), ("ring", sd[1][0])], writes=[("ps", bnk)])
                        P.add("dve", lambda e, dc=dc, bnk=bnk, c0=c0, c1=c1, W=W: e.tensor_tensor(
                            out=h[:, dc, c0:c1], in0=h[:, dc, c0:c1], in1=ps[bnk][:, 0:W], op=ALU.add),
                            reads=[("ps", bnk)] + hkeys(dc, c0, c1), writes=hkeys(dc, c0, c1))

        T0 = [(96, 512), (512, 896), (896, 1280)]
        T1 = [(128, 512), (512, 896), (896, 1280)]

        P.barrier()
        if stage >= 1:
            mixer(0)
        P.barrier()
        if stage >= 2:
            ffn_norm(0, T0)
            ffn_body(T0, dg, du, dd, DFF_D // 128, None)
        P.barrier()
        if stage >= 3:
            mixer(1)
        P.barrier()
        ffn_norm(1, T1)
        s_r = [P.dsem(), P.dsem()]
        P.add("sp", lambda e: e.dma_start(out=wrg, in_=wr.rearrange("(k p) e -> p k e", p=128)), writes=["wrg"], dsem=s_r[0])
        P.add("sp", lambda e: e.dma_start(out=selt[0:8, :, :], in_=sel_d.rearrange("k (e n) -> k e n", e=8)), writes=["selt"], dsem=s_r[1])
        for k in range(KC):
            P.add("dve", lambda e, k=k: e.tensor_scalar(out=wrg[:, k, :], in0=wrg[:, k, :], scalar1=c_gffn(1, k), scalar2=None, op0=ALU.mult),
                  reads=["wrg", "cvec"], writes=["wrg"])
        for q in (range(1, 10) if stage >= 4 else []):
            c0 = q * 128
            tile0 = 128 if q < 4 else (512 if q < 7 else 896)
            b1 = bank("all")

            def mmr(e, b1=b1, c0=c0):
                ins = None
                for k in range(KC):
                    ins = e.matmul(ps[b1][:, 0:8], lhsT=h[:, k, c0:c0 + 128], rhs=wrg[:, k, :], start=(k == 0), stop=(k == KC - 1))
                return ins
            P.add("pe", mmr, reads=["wrg"] + [("h", k, q) for k in range(KC)], writes=[("ps", b1)])
            b2 = bank("all")
            P.add("pe", lambda e, b2=b2, c0=c0: e.matmul(ps[b2][:, 0:1], lhsT=rstd[0:1, c0:c0 + 128], rhs=onesf[0:1, 0:1], start=True, stop=True),
                  reads=[("rstd", tile0), "onesf"], writes=[("ps", b2)])
            P.add("dve", lambda e, b1=b1: e.tensor_copy(out=small[:, 8:16], in_=ps[b1][:, 0:8]), reads=[("ps", b1)], writes=["raw"])
            P.add("dve", lambda e, b2=b2: e.tensor_copy(out=small[:, 29:30], in_=ps[b2][:, 0:1]), reads=[("ps", b2)], writes=["rcol"])
            P.add("dve", lambda e: e.max(out=small[:, 16:24], in_=small[:, 8:16]), reads=["raw"], writes=["top8"])
            P.add("dve", lambda e: e.tensor_tensor(out=small[:, 24:25], in0=small[:, 17:18], in1=small[:, 16:17], op=ALU.subtract),
                  reads=["top8"], writes=["diff"])
            P.add("act", lambda e: e.activation(out=small[:, 25:26], in_=small[:, 24:25], func=AF.Exp, scale=small[:, 29:30]),
                  reads=["diff", "rcol"], writes=["ex"])
            P.add("dve", lambda e: e.tensor_scalar(out=small[:, 26:27], in0=small[:, 25:26], scalar1=1.0, scalar2=None, op0=ALU.add),
                  reads=["ex"], writes=["den"])
            P.add("dve", lambda e: e.reciprocal(out=small[:, 27:28], in_=small[:, 26:27]), reads=["den"], writes=["g1"])
            P.add("dve", lambda e: e.tensor_tensor(out=small[:, 28:29], in0=small[:, 25:26], in1=small[:, 27:28], op=ALU.mult),
                  reads=["ex", "g1"], writes=["g2"])
            P.add("dve", lambda e: e.tensor_scalar(out=small[:, 32:40], in0=small[:, 8:16], scalar1=small[:, 16:17], scalar2=small[:, 27:28],
                                                   op0=ALU.is_equal, op1=ALU.mult), reads=["raw", "top8", "g1"], writes=["c1"])
            P.add("dve", lambda e: e.tensor_scalar(out=small[:, 40:48], in0=small[:, 8:16], scalar1=small[:, 17:18], scalar2=small[:, 28:29],
                                                   op0=ALU.is_equal, op1=ALU.mult), reads=["raw", "top8", "g2"], writes=["c2"])
            P.add("dve", lambda e: e.tensor_tensor(out=small[:, 40:48], in0=small[:, 40:48], in1=small[:, 32:40], op=ALU.add),
                  reads=["c1", "c2"], writes=["comb"])
            b3 = bank("all")
            P.add("pe", lambda e, b3=b3: e.transpose(out=ps[b3][0:8, 0:128], in_=small[:, 40:48], identity=identf[:, :]),
                  reads=["comb", "identf"], writes=[("ps", b3)])
            P.add("act", lambda e, b3=b3, c0=c0: e.activation(out=combT[0:8, c0 - 128:c0], in_=ps[b3][0:8, 0:128], func=AF.Copy),
                  reads=[("ps", b3)], writes=[("combT", q)])
        for ex in (range(NE) if stage >= 5 else []):
            gs = ex % 2
            for ti in range(3):
                bnk = bank("all")
                P.add("pe", lambda e, ex=ex, ti=ti, bnk=bnk: e.matmul(ps[bnk][:, 0:384], lhsT=selt[0:8, ex, :], rhs=combT[0:8, ti * 384:(ti + 1) * 384],
                                                                      start=True, stop=True),
                      reads=["selt"] + [("combT", q) for q in range(1, 10)], writes=[("ps", bnk)])
                P.add("act", lambda e, gs=gs, ti=ti, bnk=bnk: e.activation(out=gbc[:, gs, ti * 384:(ti + 1) * 384], in_=ps[bnk][:, 0:384], func=AF.Copy),
                      reads=[("ps", bnk)], writes=[("gbc", gs)])
            ffn_body(T1, mg[ex], mu[ex], md[ex], DFF_E // 128, gs)

        P.barrier()
        for (c0, c1) in T1:
            norm_stage(c0, c1, abuf[:, :, 0:416], "abuf", rstd[:, c0:c1], ("rstd", c0), None, None, None, do_xn=False)
        s_y = [P.dsem(), P.dsem()]
        outkeys = []
        for q in range(1, 10):
            c0 = q * 128
            tile0 = 128 if q < 4 else (512 if q < 7 else 896)
            sl = q % 2
            for c4 in range(4):
                for j in range(4):
                    c = c4 * 4 + j
                    P.add("dve", lambda e, c=c, j=j, c0=c0: e.scalar_tensor_tensor(out=ytmp[:, j, :], in0=h[:, c, c0:c0 + 128], scalar=c_gfin(c),
                                                                               in1=rstd[:, c0:c0 + 128], op0=ALU.mult, op1=ALU.mult),
                          reads=[("h", c, q), ("rstd", tile0), "cvec"], writes=[("ytmp", j)])
                bnk = bank("all")

                def tr(e, bnk=bnk):
                    ins = None
                    for j in range(4):
                        ins = e.transpose(out=ps[bnk][:, j * 128:(j + 1) * 128], in_=ytmp[:, j, :], identity=identf[:, :])
                    return ins
                P.add("pe", tr, reads=[("ytmp", j) for j in range(4)] + ["identf"], writes=[("ps", bnk)])
                evac(xstage[:, sl, c4 * 512:(c4 + 1) * 512], ps[bnk][:, :], reads=[("ps", bnk)], writes=[("xs", sl, c4)])
            P.add("sp", lambda e, q=q, sl=sl: e.dma_start(out=yout[(q - 1) * 128:q * 128, :], in_=xstage[:, sl, :]),
                  reads=[("xs", sl, c4) for c4 in range(4)], writes=[("yout", q)], dsem=s_y[sl])
            outkeys.append(("yout", q))
        outkeys += [k for k in ["dbg1", "dbg2", "dbg3"] if k in P.last_w]
        for l in range(2):
            outkeys += [k for k in [("npp", l), ("nps_a", l), ("nps_b", l), ("nvs", l)] if k in P.last_w]
        P.add("sp", lambda e: e.wait_ge(s_y[0].h, 0), reads=outkeys)

        P.assign()
        block = es.enter_context(nc.Block())
        P.finalize(block)
    return nc


def _band_mats(start_core):
    m = np.zeros((27, 128, 128), np.float32)
    m[0] = np.eye(128, dtype=np.float32)
    s = np.arange(128)[:, None]
    t = np.arange(128)[None, :]
    m[1] = (s <= t).astype(np.float32)
    m[2] = ((s // 8 == t // 8) & (s % 8 <= t % 8)).astype(np.float32)
    for g, w in enumerate(WINS):
        base = 3 + g * 6
        inwin = ((s <= t) & (s > t - w)).astype(np.float32)
        m[base + 0] = inwin / w - np.eye(128, dtype=np.float32)
        m[base + 1] = ((s - 128) > (t - w)).astype(np.float32) / w
        if start_core:
            cnt = np.minimum(t + 1, w).astype(np.float32)
            m[base + 2] = inwin / cnt - np.eye(128, dtype=np.float32)
        else:
            m[base + 2] = m[base + 0]
        sb, st_ = s // 8, s % 8
        tb, tt = t // 8, t % 8
        m[base + 3] = ((sb == tb) & (st_ <= tt) & (st_ > tt - w)).astype(np.float32) / w - np.eye(128, dtype=np.float32)
        for j in range(2):
            rows = np.arange(128)[:, None]
            rb = rows // 15 + 8 * j
            rr = rows % 15
            ok = (rows < 120) & (rb == tb) & (rr > 15 + tt - w)
            m[base + 4 + j] = ok.astype(np.float32) / w
    return np.ascontiguousarray(m.transpose(1, 0, 2))


_NC_CACHE = {}


def kernel(x_prompt, x_sample, state_pool, g_mix, w_in, g_v, w_pool, pool_scale, w_s, b_s, w_out, g_ffn,
           dense_w_gate, dense_w_up, dense_w_down, w_router, moe_w_gate, moe_w_up, moe_w_down, g_final):
    f = lambda a: np.ascontiguousarray(np.asarray(a, dtype=np.float32))
    x_prompt, x_sample, state_pool = f(x_prompt), f(x_sample), f(state_pool)
    w_s, b_s = f(w_s), f(b_s)
    if "nc" not in _NC_CACHE:
        _NC_CACHE["nc"] = build_program()
    nc = _NC_CACHE["nc"]

    def pp(v):
        return np.asarray(v, np.float32).reshape(-1, 128).T

    cvec = np.concatenate([pp(g_mix[0]), pp(g_mix[1]), pp(pool_scale[0]), pp(pool_scale[1]),
                           pp(g_ffn[0]), pp(g_ffn[1]), pp(g_final)], axis=1)
    cvec = np.ascontiguousarray(cvec, dtype=np.float32)
    gvbc = np.ascontiguousarray(np.broadcast_to(np.asarray(g_v, np.float32)[:, None, :], (2, 128, DP)))
    wsT = np.ascontiguousarray(w_s.transpose(0, 3, 1, 2))
    blk = w_s[:, :, :8, :8].transpose(0, 3, 1, 2)
    wsbig = np.ascontiguousarray(np.tile(blk, (1, 16, 1, 16)))
    bsrow = np.zeros((2, 1, 2048), np.float32)
    bsrow[:, 0, :1024] = b_s.reshape(2, 1024)
    bsrow[:, 0, 1024:] = np.tile(b_s[:, :, None, :8], (1, 1, 16, 1)).reshape(2, 1024)
    identf = np.eye(128, dtype=np.float32)
    sel = np.zeros((8, 8, 128), np.float32)
    for e in range(8):
        sel[e, e, :] = 1.0
    sel = sel.reshape(8, 1024)
    shared = dict(w_in=f(w_in), w_out=f(w_out), w_pool=f(w_pool), dg=f(dense_w_gate)[0], du=f(dense_w_up)[0], dd=f(dense_w_down)[0],
                  mg=f(moe_w_gate)[0], mu=f(moe_w_up)[0], md=f(moe_w_down)[0], wr=f(w_router)[0], wsT=wsT, wsbig=wsbig,
                  cvec=cvec, gvbc=gvbc, bsrow=bsrow, identf=identf, sel=sel)
    cm = [_band_mats(True), _band_mats(False)]
    in_maps = []
    for c in range(NCORES):
        b, half = c // 2, c % 2
        s0 = half * 1024
        xin = np.zeros((TT, D), np.float32)
        if half == 1:
            xin[0:128] = x_prompt[b, s0 - 128:s0]
        xin[128:1152] = x_prompt[b, s0:s0 + 1024]
        xin[1152:1280] = x_sample[16 * c:16 * c + 16].reshape(128, D)
        m = dict(shared)
        m["xin"] = xin
        m["spool"] = np.ascontiguousarray(state_pool[:, 16 * c:16 * c + 16].reshape(2, 240, DP))
        m["cmat"] = cm[0] if half == 0 else cm[1]
        in_maps.append(m)
    res = run_bass_kernel_spmd(nc, in_maps, core_ids=list(range(NCORES)))
    R = res.results
    y_prompt = np.zeros((4, 2048, D), np.float32)
    y_sample = np.zeros((128, 8, D), np.float32)
    npp = np.zeros((2, 4, 15, DP), np.float32)
    nps = np.zeros((2, 128, 15, DP), np.float32)
    nvs = np.zeros((2, 128, 8, DP), np.float32)
    for c in range(NCORES):
        b, half = c // 2, c % 2
        yo = np.asarray(R[c]["yout"])
        y_prompt[b, half * 1024:(half + 1) * 1024] = yo[:1024]
        y_sample[16 * c:16 * c + 16] = yo[1024:].reshape(16, 8, D)
        if half == 1:
            npp[:, b] = np.asarray(R[c]["npp"])
        nps[:, 16 * c:16 * c + 16, 0:7] = np.asarray(R[c]["nps_a"])
        nps[:, 16 * c:16 * c + 16, 7:15] = np.asarray(R[c]["nps_b"]).reshape(2, 16, 8, DP)
        nvs[:, 16 * c:16 * c + 16] = np.asarray(R[c]["nvs"]).reshape(2, 16, 8, DP)
    return (y_prompt, y_sample, npp, nps, nvs)
```
